# Optimizing a Trainium2 kernel written in Bass

```python
import math
import jax, jax.numpy as jnp
from jax import lax
import numpy as np

D_MODEL = 2048
BATCH = 2
SEQ = 16384
DEPTH = 1

ATTN_HEADS = 8
HEAD_DIM = 128
ATTN_WIDTH = ATTN_HEADS * HEAD_DIM
IDX_HEADS = 8
IDX_DIM = 64
DSA_TOPK = 256
Q_BLOCK = 128
GMLP_GROUPS = 8
GMLP_GROUP_CH = 128
GMLP_WIDTH = GMLP_GROUPS * GMLP_GROUP_CH
CHUNK = 128
N_BRANCH = 2
N_KEYS = 128
N_EXPERTS = N_KEYS * N_KEYS
PEER_HEADS = 8
PEER_QDIM = 256
PEER_TOPK = 16
PEER_TOK_BLOCK = 128
EPS = 1e-6

kernel_name = "hybrid_dsa_gmlp_peer_block"


def rms_norm(x, g):
    xf = x.astype(jnp.float32)
    y = xf * lax.rsqrt(jnp.mean(xf * xf, axis=-1, keepdims=True) + EPS)
    return (y * g.astype(jnp.float32)).astype(x.dtype)


def dsa_attention(q, k, v, q_idx, k_idx, w_idx):
    B, S = q.shape[0], q.shape[1]
    n_sel = min(DSA_TOPK, S // 4)
    nb = S // Q_BLOCK

    def to_blocks(a):
        return jnp.swapaxes(a.reshape((B, nb, Q_BLOCK) + a.shape[2:]), 0, 1)

    q_pos = jnp.arange(S, dtype=jnp.int32).reshape(nb, Q_BLOCK)
    key_pos = jnp.arange(S, dtype=jnp.int32)
    bidx = jnp.arange(B, dtype=jnp.int32)[:, None, None]
    idx_scale = IDX_DIM ** -0.5
    attn_scale = HEAD_DIM ** -0.5

    def block(args):
        qb, qib, wb, qpos = args
        dots = jnp.einsum('bqhd,bsd->bqhs', qib, k_idx).astype(jnp.float32) * idx_scale
        score = jnp.einsum('bqhs,bqh->bqs', jax.nn.relu(dots), wb.astype(jnp.float32))
        causal = key_pos[None, None, :] <= qpos[None, :, None]
        score = jnp.where(causal, score, -jnp.inf)
        _, sel = lax.top_k(score, n_sel)
        valid = sel <= qpos[None, :, None]
        k_sel = k[bidx, sel]
        v_sel = v[bidx, sel]
        logits = jnp.einsum('bqhd,bqkhd->bqhk', qb, k_sel).astype(jnp.float32) * attn_scale
        logits = jnp.where(valid[:, :, None, :], logits, -jnp.inf)
        p = jax.nn.softmax(logits, axis=-1).astype(v.dtype)
        return jnp.einsum('bqhk,bqkhd->bqhd', p, v_sel)

    out = lax.map(block, (to_blocks(q), to_blocks(q_idx), to_blocks(w_idx), q_pos))
    return jnp.swapaxes(out, 0, 1).reshape(B, S, ATTN_WIDTH)


def chunked_spatial_gating(u, v, norm_g, w_s, b_s):
    B, S = u.shape[0], u.shape[1]
    nc = S // CHUNK
    u = jax.nn.gelu(u)
    v = rms_norm(jax.nn.gelu(v), norm_g)
    v = v.reshape(B, nc, CHUNK, GMLP_GROUPS, GMLP_GROUP_CH)
    mask = jnp.tril(jnp.ones((CHUNK, CHUNK), dtype=w_s.dtype))
    z = jnp.einsum('gts,bnsgc->bntgc', w_s * mask[None], v)
    z = z + jnp.swapaxes(b_s, 0, 1)[None, None, :, :, None]
    return u * z.reshape(B, S, GMLP_WIDTH)


def peer(h, w_q, sub_keys, u_emb, v_emb):
    B, S, D = h.shape
    tokens = h.reshape(-1, PEER_TOK_BLOCK, D)
    half = PEER_QDIM // 2

    def block(hb):
        T = hb.shape[0]
        q = (hb @ w_q).reshape(T, PEER_HEADS, 2, half)
        s = jnp.einsum('thpd,hpnd->thpn', q, sub_keys).astype(jnp.float32)
        top_s, top_i = lax.top_k(s, PEER_TOPK)
        cand_s = top_s[:, :, 0, :, None] + top_s[:, :, 1, None, :]
        cand_i = top_i[:, :, 0, :, None] * N_KEYS + top_i[:, :, 1, None, :]
        best_s, best_j = lax.top_k(cand_s.reshape(T, PEER_HEADS, PEER_TOPK * PEER_TOPK), PEER_TOPK)
        expert = jnp.take_along_axis(cand_i.reshape(T, PEER_HEADS, PEER_TOPK * PEER_TOPK), best_j, axis=-1)
        gate = jax.nn.softmax(best_s, axis=-1).astype(hb.dtype)
        u_sel = u_emb[expert]
        v_sel = v_emb[expert]
        act = jax.nn.gelu(jnp.einsum('td,thkd->thk', hb, u_sel))
        return jnp.einsum('thk,thkd->td', gate * act, v_sel)

    return lax.map(block, tokens).reshape(B, S, D)


def setup_inputs(seed: int = 0) -> dict:
    key = jax.random.key(seed)
    ks = jax.random.split(key, 16)
    L = DEPTH
    n_in = 3 * ATTN_WIDTH + IDX_HEADS * IDX_DIM + IDX_DIM + IDX_HEADS + 2 * GMLP_WIDTH + N_BRANCH * D_MODEL

    def nrm(k, shape, scale):
        return jax.random.normal(k, shape, jnp.float32) * scale

    return {
        "x": nrm(ks[0], (BATCH, SEQ, D_MODEL), 1.0),
        "ln_mix_g": 1.0 + nrm(ks[1], (L, D_MODEL), 0.02),
        "w_in": nrm(ks[2], (L, D_MODEL, n_in), D_MODEL ** -0.5),
        "gmlp_norm_g": 1.0 + nrm(ks[3], (L, GMLP_WIDTH), 0.02),
        "w_spatial": nrm(ks[4], (L, GMLP_GROUPS, CHUNK, CHUNK), CHUNK ** -0.5),
        "b_spatial": nrm(ks[5], (L, GMLP_GROUPS, CHUNK), 0.02),
        "w_branch_attn": nrm(ks[6], (L, ATTN_WIDTH, D_MODEL), ATTN_WIDTH ** -0.5),
        "w_branch_gmlp": nrm(ks[7], (L, GMLP_WIDTH, D_MODEL), GMLP_WIDTH ** -0.5),
        "w_out": nrm(ks[8], (L, D_MODEL, D_MODEL), D_MODEL ** -0.5),
        "ln_ffn_g": 1.0 + nrm(ks[9], (L, D_MODEL), 0.02),
        "peer_w_q": nrm(ks[10], (L, D_MODEL, PEER_HEADS * PEER_QDIM), D_MODEL ** -0.5),
        "peer_sub_keys": nrm(ks[11], (L, PEER_HEADS, 2, N_KEYS, PEER_QDIM // 2), (PEER_QDIM // 2) ** -0.5),
        "peer_u": nrm(ks[12], (L, N_EXPERTS, D_MODEL), D_MODEL ** -0.5),
        "peer_v": nrm(ks[13], (L, N_EXPERTS, D_MODEL), (PEER_HEADS * PEER_TOPK) ** -0.5),
        "ln_final_g": 1.0 + nrm(ks[14], (D_MODEL,), 0.02),
    }


def reference(x, ln_mix_g, w_in, gmlp_norm_g, w_spatial, b_spatial, w_branch_attn, w_branch_gmlp,
              w_out, ln_ffn_g, peer_w_q, peer_sub_keys, peer_u, peer_v, ln_final_g):
    B, S, _ = x.shape
    sizes = [ATTN_WIDTH, ATTN_WIDTH, ATTN_WIDTH, IDX_HEADS * IDX_DIM, IDX_DIM, IDX_HEADS,
             GMLP_WIDTH, GMLP_WIDTH, D_MODEL, D_MODEL]
    split_at = [int(c) for c in np.cumsum(sizes)[:-1]]
    for l in range(DEPTH):
        h = rms_norm(x, ln_mix_g[l])
        proj = h @ w_in[l]
        q, k, v, qi, ki, wi, gu, gv, ga, gb = jnp.split(proj, split_at, axis=-1)
        q = q.reshape(B, S, ATTN_HEADS, HEAD_DIM)
        k = k.reshape(B, S, ATTN_HEADS, HEAD_DIM)
        v = v.reshape(B, S, ATTN_HEADS, HEAD_DIM)
        qi = qi.reshape(B, S, IDX_HEADS, IDX_DIM)
        wi = wi * (IDX_HEADS ** -0.5)
        y_a = dsa_attention(q, k, v, qi, ki, wi)
        y_b = chunked_spatial_gating(gu, gv, gmlp_norm_g[l], w_spatial[l], b_spatial[l])
        mixed = jax.nn.sigmoid(ga) * (y_a @ w_branch_attn[l]) + jax.nn.sigmoid(gb) * (y_b @ w_branch_gmlp[l])
        x = x + mixed @ w_out[l]
        x = x + peer(rms_norm(x, ln_ffn_g[l]), peer_w_q[l], peer_sub_keys[l], peer_u[l], peer_v[l])
    return rms_norm(x, ln_final_g)
```

```python
from contextlib import ExitStack
import numpy as np
import concourse.bass as bass
import concourse.mybir as mybir
from concourse.bass_utils import run_bass_kernel_spmd

F32 = mybir.dt.float32
BF16 = mybir.dt.bfloat16
U32 = mybir.dt.uint32
AF = mybir.ActivationFunctionType
OP = mybir.AluOpType
AX = mybir.AxisListType

D = 2048
KC = 16
NE = 16384
EPS = 1e-6
NEG = -1.0e30
C_Q, C_K, C_V, C_QI, C_KI, C_WI, C_GU, C_GV, C_GA, C_GB = 0, 1024, 2048, 3072, 3584, 3648, 3656, 4680, 5704, 7752
NBIS = 21


class Dep:
    __slots__ = ("w", "r")

    def __init__(self):
        self.w = None
        self.r = []


class Buf:
    def __init__(self, t):
        self.t = t
        self.d = Dep()

    def __getitem__(self, idx):
        return self.t[idx]


class KB:
    def __init__(self, nc, stack, needed=None, n_dma_sems=20):
        self.nc = nc
        self.dry = needed is None
        self.engs = {"pe": nc.tensor, "act": nc.scalar, "dve": nc.vector,
                     "pool": nc.gpsimd, "sp": nc.sync}
        self.sem = {}
        self.cnt = {}
        for e in self.engs:
            self.sem[e] = stack.enter_context(nc.semaphore("s_" + e))
            self.cnt[e] = 0
        self.dsem = {}
        self.dnext = {}
        for q in ("sp", "pool", "act"):
            lst = []
            for i in range(n_dma_sems):
                key = f"d_{q}_{i}"
                self.sem[key] = stack.enter_context(nc.semaphore(key))
                self.cnt[key] = 0
                lst.append(key)
            self.dsem[q] = lst
            self.dnext[q] = 0
        self.seen = {e: {} for e in self.engs}
        self.snap = {}
        self.want = {e: set() for e in self.engs}
        if needed is not None:
            self.rank = {e: {n: i + 1 for i, n in enumerate(sorted(needed[e]))} for e in self.engs}
        self.n_inst = 0

    def _wait(self, e, key, val):
        if val <= 0 or self.seen[e].get(key, 0) >= val:
            return
        se = self.seen[e]
        se[key] = val
        sn = self.snap.get((key, val))
        if sn:
            for kk, vv in sn.items():
                if se.get(kk, 0) < vv:
                    se[kk] = vv
        self.n_inst += 1
        if key in self.engs:
            if self.dry:
                self.want[key].add(val)
            else:
                self.engs[e].wait_ge(self.sem[key], self.rank[key][val])
        elif not self.dry:
            self.engs[e].wait_ge(self.sem[key], val)

    def _deps(self, e, reads, writes, skip_self):
        need = {}

        def add(tok):
            if skip_self and tok[0] == e:
                return
            if need.get(tok[0], 0) < tok[1]:
                need[tok[0]] = tok[1]

        for b in reads:
            if b.d.w is not None:
                add(b.d.w)
        for b in writes:
            d = b.d
            if d.w is not None:
                add(d.w)
            for rr in d.r:
                add(rr)
        for key, val in need.items():
            self._wait(e, key, val)

    def _mark(self, tok, reads, writes):
        for b in reads:
            d = b.d
            d.r.append(tok)
            if len(d.r) > 48:
                mx = {}
                for kk, v in d.r:
                    if mx.get(kk, 0) < v:
                        mx[kk] = v
                d.r = list(mx.items())
        for b in writes:
            b.d.w = tok
            b.d.r = []

    def op(self, e, fn, reads=(), writes=(), inc=True, skip_self=False):
        self._deps(e, reads, writes, skip_self)
        self.cnt[e] += 1
        n = self.cnt[e]
        self.n_inst += 1
        if not self.dry:
            ins = fn()
            if n in self.rank[e]:
                ins.then_inc(self.sem[e], 1)
        tok = (e, n)
        sn = dict(self.seen[e])
        sn[e] = n - 1
        self.snap[tok] = sn
        self._mark(tok, reads, writes)

    def dma(self, q, out, in_, reads=(), writes=()):
        lst = self.dsem[q]
        key = lst[self.dnext[q] % len(lst)]
        self.dnext[q] += 1
        self._wait(q, key, self.cnt[key])
        self._deps(q, reads, writes, False)
        self.cnt[key] += 16
        if not self.dry:
            ins = self.engs[q].dma_start(out=out, in_=in_)
            ins.then_inc(self.sem[key], 16)
        tok = (key, self.cnt[key])
        self.snap[tok] = dict(self.seen[q])
        self._mark(tok, reads, writes)
        self.n_inst += 1

    def barrier(self):
        for e in self.engs:
            for key in self.sem:
                if key != e:
                    self._wait(e, key, self.cnt[key])
        self.snap = {}


def build(S, phases=None, expose=(), needed=None):
    if phases is None:
        phases = {"0", "1a", "1b", "2a", "2b", "2c", "3a", "3b", "4"}
    NT = S // 128
    NO = NT // 4
    NST = S // 512
    nc = bass.Bass("TRN2", target_bir_lowering=False)

    def din(name, shape, dt=F32):
        return nc.dram_tensor(name, shape, dt, kind="ExternalInput").ap()

    def dsc(name, shape, dt):
        kind = "ExternalOutput" if name in expose else "Internal"
        return nc.dram_tensor(name, shape, dt, kind=kind).ap()

    x_all = din("x_all", [S, D])
    x_own = din("x_own", [NO * 128, D])
    cmask_d = din("cmask", [128, 512])
    tril_d = din("tril", [128, 128])
    ident_d = din("ident", [128, 128])
    iota_d = din("iota", [128, 128])
    g_mix_d = din("g_mix", [128, D])
    g_ffn_d = din("g_ffn", [128, D])
    g_fin_d = din("g_fin", [128, D])
    g_gm_d = din("g_gm", [128, 1024])
    w_in = din("w_in", [D, 9800])
    wsT_d = din("wsT", [8, 128, 128])
    bT_d = din("bT", [128, 8])
    w_ba = din("w_ba", [1024, D])
    w_bg = din("w_bg", [1024, D])
    w_o = din("w_o", [D, D])
    w_pq = din("w_pq", [D, D])
    skT_d = din("skT", [16, 128, 128])
    if "0" in phases:
        uT_d = din("uT", [D, NE])
        v_d = din("pv", [NE, D])
    out_d = nc.dram_tensor("out", [NO * 128, D], F32, kind="ExternalOutput").ap()

    Ubf = dsc("Ubf", [D, NE], BF16)
    Vbf = dsc("Vbf", [NE, D], BF16)
    KT = dsc("KT", [8, 128, S], BF16)
    Vs = dsc("Vs", [S, 8 * 129], BF16)
    hTo = dsc("hTo", [NO, 128, KC, 128], BF16)
    qT = dsc("qT", [NO, 128, 8, 128], BF16)
    qiT = dsc("qiT", [NO, 64, 8, 128], BF16)
    wi_s = dsc("wi_s", [NO, 128, 8], F32)
    gu_s = dsc("gu_s", [NO, 128, 1024], BF16)
    gv_s = dsc("gv_s", [NO, 128, 1024], BF16)
    sga_s = dsc("sga_s", [NO, 128, D], BF16)
    sgb_s = dsc("sgb_s", [NO, 128, D], BF16)
    yaT = dsc("yaT", [NO, 128, 8, 128], BF16)
    x1_s = dsc("x1_s", [NO, 128, D], F32)
    hn2T = dsc("hn2T", [NO, 128, KC, 128], BF16)
    G_s = dsc("G_s", [NO, 128, 128, 128], BF16)
    kiT_s = dsc("kiT_s", [64, S], BF16)
    mixT_s = dsc("mixT_s", [NO, 128, KC, 128], BF16)
    s_s = dsc("s_s", [NO, 128, 16, 128], F32)

    with ExitStack() as top:
        k = KB(nc, top, needed)

        def SB(st, name, shape, dt):
            return Buf(st.enter_context(nc.sbuf_tensor("sb_" + name, shape, dt)))

        def PS(st, name, shape, dt):
            return Buf(st.enter_context(nc.psum_tensor("ps_" + name, shape, dt)))

        pb = [PS(top, f"pb{i}", [128, 512], F32) for i in range(7)]
        ptr = PS(top, "ptr", [128, 1024], BF16)

        ident = SB(top, "ident", [128, 128], BF16)
        identf = SB(top, "identf", [128, 128], F32)
        junk = SB(top, "junk", [128, 8], BF16)
        k.dma("sp", identf[:], ident_d, writes=[identf])
        k.op("dve", lambda: nc.vector.tensor_copy(out=ident[:], in_=identf[:]), [identf], [ident])

        rr = [0]

        def evac(out, in_, reads, writes):
            rr[0] += 1
            if rr[0] % 2:
                k.op("act", lambda: nc.scalar.copy(out=out, in_=in_), reads, writes)
            else:
                k.op("dve", lambda: nc.vector.tensor_copy(out=out, in_=in_), reads, writes)

        def cast(e, out, in_, reads, writes):
            if e == "act":
                k.op("act", lambda: nc.scalar.copy(out=out, in_=in_), reads, writes)
            elif e == "pool":
                k.op("pool", lambda: nc.gpsimd.tensor_copy(out=out, in_=in_), reads, writes)
            else:
                k.op("dve", lambda: nc.vector.tensor_copy(out=out, in_=in_), reads, writes)

        def mm(ps, out_ap, lhsT, rhs, start, stop, reads, inc=None):
            if inc is None:
                inc = stop
            k.op("pe", lambda: nc.tensor.matmul(out_ap, lhsT=lhsT, rhs=rhs, start=start, stop=stop),
                 reads, [ps], inc=inc, skip_self=True)

        def mm_acc(ps, out_ap, lhsT, rhs, reads, inc):
            k.op("pe", lambda: nc.tensor.matmul(out_ap, lhsT=lhsT, rhs=rhs, start=False, stop=False,
                                                skip_group_check=True),
                 reads, [ps], inc=inc, skip_self=True)

        def transp(ps, out_ap, in_ap, idt, reads, inc=True):
            k.op("pe", lambda: nc.tensor.transpose(out=out_ap, in_=in_ap, identity=idt[:]),
                 list(reads) + [idt], [ps], inc=inc, skip_self=True)

        def load_w(st_bufs, dst, dst_c0, src_cols, ncols, engs=("pool", "dve")):
            nk = src_cols.shape[0] // 128
            for kc in range(nk):
                sg = st_bufs[kc % len(st_bufs)]
                k.dma("sp", sg[:, 0:ncols], src_cols[kc * 128:(kc + 1) * 128, :], writes=[sg])
                cast(engs[kc % len(engs)], dst[:, kc, dst_c0:dst_c0 + ncols], sg[:, 0:ncols], [sg], [dst])

        def rms_rows(st, xt, ssq, rstd, sqj):
            k.op("act", lambda: nc.scalar.activation(out=sqj[:], in_=xt[:], func=AF.Square, accum_out=ssq[:, 0:1]),
                 [xt], [sqj, ssq])
            k.op("dve", lambda: nc.vector.tensor_scalar(out=rstd[:], in0=ssq[:, 0:1], scalar1=1.0 / D, scalar2=EPS,
                                                        op0=OP.mult, op1=OP.add), [ssq], [rstd])
            k.op("act", lambda: nc.scalar.activation(out=rstd[:], in_=rstd[:], func=AF.Sqrt), [rstd], [rstd])
            k.op("dve", lambda: nc.vector.reciprocal(out=rstd[:], in_=rstd[:]), [rstd], [rstd])

        def norm_transpose(xt, gbc, rstd, hn, hT_buf, hT_view_fn):
            k.op("dve", lambda: nc.vector.scalar_tensor_tensor(out=hn[:], in0=xt[:], scalar=rstd[:, 0:1], in1=gbc[:],
                                                               op0=OP.mult, op1=OP.mult), [xt, rstd, gbc], [hn])
            for half in range(2):
                for j in range(8):
                    kc = half * 8 + j
                    transp(ptr, ptr[:, j * 128:(j + 1) * 128], hn[:, kc * 128:(kc + 1) * 128], ident, [hn],
                           inc=(j == 7))
                evac(hT_view_fn(half * 8, 8), ptr[:].rearrange("p (j t) -> p j t", j=8), [ptr], [hT_buf])

        def p0_gen(stg, cb, engs):
            it = 0
            for src, dst, rows, cols in ((uT_d, Ubf, D, NE), (v_d, Vbf, NE, D)):
                for r0 in range(0, rows, 128):
                    for c0 in range(0, cols, 4096):
                        w = min(4096, cols - c0)
                        s_, c_ = stg[it % 2], cb[it % 2]
                        k.dma("sp", s_[:, 0:w], src[r0:r0 + 128, c0:c0 + w], writes=[s_])
                        cast(engs[it % len(engs)], c_[:, 0:w], s_[:, 0:w], [s_], [c_])
                        k.dma("pool", dst[r0:r0 + 128, c0:c0 + w], c_[:, 0:w], reads=[c_])
                        it += 1
                        yield

        if "0" in phases and "1a" not in phases:
            with ExitStack() as ph:
                stg = [SB(ph, f"p0s{i}", [128, 4096], F32) for i in range(2)]
                cb = [SB(ph, f"p0c{i}", [128, 4096], BF16) for i in range(2)]
                for _ in p0_gen(stg, cb, ("pool", "dve", "act")):
                    pass
            k.barrier()

        if "1a" in phases:
            with ExitStack() as ph:
                wkv = SB(ph, "wkv", [128, KC, 2112], BF16)
                stg = [SB(ph, f"p1s{i}", [128, 1024], F32) for i in range(2)]
                gbc = SB(ph, "gbc", [128, D], F32)
                k.dma("sp", gbc[:], g_mix_d, writes=[gbc])
                load_w(stg, wkv, 0, w_in[:, C_K:C_K + 1024], 1024)
                load_w(stg, wkv, 1024, w_in[:, C_V:C_V + 1024], 1024)
                load_w(stg, wkv, 2048, w_in[:, C_KI:C_KI + 64], 64)
                xin = [SB(ph, f"xin{i}", [128, D], F32) for i in range(2)]
                sqj = SB(ph, "sqj", [128, D], BF16)
                hn = [SB(ph, f"hn{i}", [128, D], BF16) for i in range(2)]
                ssq = [SB(ph, f"ssq{i}", [128, 1], F32) for i in range(2)]
                rstd = [SB(ph, f"rstd{i}", [128, 1], F32) for i in range(2)]
                hT = [SB(ph, f"hT{i}", [128, KC, 512], BF16) for i in range(2)]
                ksb = [SB(ph, f"ksb{i}", [128, 512], BF16) for i in range(2)]
                vsb = [SB(ph, f"vsb{i}", [128, 8, 129], BF16) for i in range(2)]
                for v_ in vsb:
                    k.op("pool", lambda v_=v_: nc.gpsimd.memset(v_[:], 1.0), [], [v_])
                kib = [SB(ph, f"kib{i}", [64, 512], BF16) for i in range(2)]
                n_e = 0
                g0 = None
                if "0" in phases:
                    p0s = [SB(ph, f"p0s{i}", [128, 4096], F32) for i in range(2)]
                    p0c = [SB(ph, f"p0c{i}", [128, 4096], BF16) for i in range(2)]
                    g0 = p0_gen(p0s, p0c, ("pool",))
                    per0 = -(-192 // NST)
                def tick(n):
                    if g0 is not None:
                        for _ in range(n):
                            next(g0, None)

                ticks = [per0 // 6 + (1 if j < per0 % 6 else 0) for j in range(6)] if g0 is not None else [0] * 6
                for st_i in range(NST):
                    hTb = hT[st_i % 2]
                    for ts in range(4):
                        tile = st_i * 4 + ts
                        xt = xin[tile % 2]
                        k.dma("sp", xt[:], x_all[tile * 128:(tile + 1) * 128, :], writes=[xt])
                        rms_rows(ph, xt, ssq[tile % 2], rstd[tile % 2], sqj)
                        norm_transpose(xt, gbc, rstd[tile % 2], hn[tile % 2], hTb,
                                       lambda k0, n, ts=ts, hTb=hTb: hTb[:, k0:k0 + n, ts * 128:(ts + 1) * 128])
                        tick(ticks[ts])
                    for h in range(8):
                        ps = pb[n_e % 4]
                        for kc in range(KC):
                            mm(ps, ps[:, :], wkv[:, kc, h * 128:(h + 1) * 128], hTb[:, kc, :], kc == 0, kc == KC - 1,
                               [wkv, hTb])
                        kb_ = ksb[n_e % 2]
                        evac(kb_[:], ps[:, :], [ps], [kb_])
                        k.dma("pool", KT[h, :, st_i * 512:(st_i + 1) * 512], kb_[:], reads=[kb_])
                        n_e += 1
                    tick(ticks[4])
                    for ts in range(4):
                        vb_ = vsb[ts % 2]
                        for half in range(2):
                            ps = pb[n_e % 4]
                            for kc in range(KC):
                                mm(ps, ps[:, :], hTb[:, kc, ts * 128:(ts + 1) * 128],
                                   wkv[:, kc, 1024 + half * 512:1024 + (half + 1) * 512], kc == 0, kc == KC - 1,
                                   [wkv, hTb])
                            evac(vb_[:, half * 4:(half + 1) * 4, 0:128], ps[:, :].rearrange("p (h d) -> p h d", h=4),
                                 [ps], [vb_])
                            n_e += 1
                        tile = st_i * 4 + ts
                        k.dma("pool", Vs[tile * 128:(tile + 1) * 128, :], vb_[:].rearrange("p h d -> p (h d)"),
                              reads=[vb_])
                    tick(ticks[5])
                    ps = pb[n_e % 4]
                    for kc in range(KC):
                        mm(ps, ps[0:64, :], wkv[:, kc, 2048:2112], hTb[:, kc, :], kc == 0, kc == KC - 1, [wkv, hTb])
                    kb_ = kib[st_i % 2]
                    evac(kb_[:], ps[0:64, :], [ps], [kb_])
                    k.dma("pool", kiT_s[:, st_i * 512:(st_i + 1) * 512], kb_[:], reads=[kb_])
                    n_e += 1
                if g0 is not None:
                    for _ in g0:
                        pass
            k.barrier()

        if "1b" in phases:
            with ExitStack() as ph:
                gbc = SB(ph, "gbc1", [128, D], F32)
                k.dma("sp", gbc[:], g_mix_d, writes=[gbc])
                xin = [SB(ph, f"b_xin{i}", [128, D], F32) for i in range(2)]
                sqj = SB(ph, "b_sqj", [128, D], BF16)
                hn = [SB(ph, f"b_hn{i}", [128, D], BF16) for i in range(2)]
                ssq = [SB(ph, f"b_ssq{i}", [128, 1], F32) for i in range(2)]
                rstd = [SB(ph, f"b_rstd{i}", [128, 1], F32) for i in range(2)]
                hTs = [SB(ph, f"b_hT{i}", [128, KC, 128], BF16) for i in range(2)]
                for i in range(NO):
                    xt = xin[i % 2]
                    hTb = hTs[i % 2]
                    k.dma("sp", xt[:], x_own[i * 128:(i + 1) * 128, :], writes=[xt])
                    rms_rows(ph, xt, ssq[i % 2], rstd[i % 2], sqj)
                    norm_transpose(xt, gbc, rstd[i % 2], hn[i % 2], hTb,
                                   lambda k0, n, hTb=hTb: hTb[:, k0:k0 + n, :])
                    k.dma("pool", hTo[i], hTb[:], reads=[hTb])
                k.barrier()
                stg = [SB(ph, f"b_s{i}", [128, 2048], F32) for i in range(2)]
                wg = SB(ph, "b_wg", [128, KC, 2048], BF16)
                osb = [SB(ph, f"b_o{i}", [128, 2048], BF16) for i in range(2)]
                osf = [SB(ph, f"b_of{i}", [128, 8], F32) for i in range(2)]
                n_e = 0
                load_w(stg, wg, 0, w_in[:, C_Q:C_Q + 1024], 1024)
                load_w(stg, wg, 1024, w_in[:, C_QI:C_QI + 512], 512)
                load_w(stg, wg, 1536, w_in[:, C_WI:C_WI + 8], 8)
                for i in range(NO):
                    hTb = hTs[i % 2]
                    k.dma("sp", hTb[:], hTo[i], writes=[hTb])
                    ob = osb[i % 2]
                    for hq in range(2):
                        ps = pb[n_e % 4]
                        n_e += 1
                        for hh in range(4):
                            h = hq * 4 + hh
                            for kc in range(KC):
                                mm(ps, ps[:, hh * 128:(hh + 1) * 128], wg[:, kc, h * 128:(h + 1) * 128], hTb[:, kc, :],
                                   kc == 0, kc == KC - 1, [wg, hTb], inc=(kc == KC - 1 and hh == 3))
                        evac(ob[:, hq * 512:(hq + 1) * 512], ps[:, :], [ps], [ob])
                    k.dma("pool", qT[i], ob[:, 0:1024].rearrange("p (h t) -> p h t", h=8), reads=[ob])
                    for hq in range(2):
                        ps = pb[n_e % 4]
                        n_e += 1
                        for hh in range(4):
                            h = hq * 4 + hh
                            for kc in range(KC):
                                mm(ps, ps[0:64, hh * 128:(hh + 1) * 128], wg[:, kc, 1024 + h * 64:1024 + (h + 1) * 64],
                                   hTb[:, kc, :], kc == 0, kc == KC - 1, [wg, hTb], inc=(kc == KC - 1 and hh == 3))
                        evac(ob[0:64, 1024 + hq * 512:1024 + (hq + 1) * 512], ps[0:64, :], [ps], [ob])
                    k.dma("pool", qiT[i], ob[0:64, 1024:2048].rearrange("p (h t) -> p h t", h=8), reads=[ob])
                    ps = pb[n_e % 4]
                    n_e += 1
                    for kc in range(KC):
                        mm(ps, ps[:, 0:8], hTb[:, kc, :], wg[:, kc, 1536:1544], kc == 0, kc == KC - 1, [wg, hTb])
                    of = osf[i % 2]
                    evac(of[:], ps[:, 0:8], [ps], [of])
                    k.dma("pool", wi_s[i], of[:], reads=[of])
                for (c0, dsts, fn) in ((C_GU, (gu_s, gv_s), AF.Gelu_apprx_tanh), (C_GA, (sga_s,), AF.Sigmoid),
                                       (C_GB, (sgb_s,), AF.Sigmoid)):
                    load_w(stg, wg, 0, w_in[:, c0:c0 + 2048], 2048)
                    for i in range(NO):
                        hTb = hTs[i % 2]
                        k.dma("sp", hTb[:], hTo[i], writes=[hTb])
                        ob = osb[i % 2]
                        for cc in range(4):
                            ps = pb[n_e % 4]
                            n_e += 1
                            for kc in range(KC):
                                mm(ps, ps[:, :], hTb[:, kc, :], wg[:, kc, cc * 512:(cc + 1) * 512], kc == 0,
                                   kc == KC - 1, [wg, hTb])
                            k.op("act", lambda ps=ps, ob=ob, cc=cc, fn=fn: nc.scalar.activation(
                                out=ob[:, cc * 512:(cc + 1) * 512], in_=ps[:, :], func=fn), [ps], [ob])
                        if len(dsts) == 2:
                            k.dma("pool", dsts[0][i], ob[:, 0:1024], reads=[ob])
                            k.dma("pool", dsts[1][i], ob[:, 1024:2048], reads=[ob])
                        else:
                            k.dma("pool", dsts[0][i], ob[:, :], reads=[ob])
            k.barrier()

        if "2a" in phases:
            with ExitStack() as ph:
                score = SB(ph, "score", [128, S], F32)
                kiT = SB(ph, "kiT", [64, S], BF16)
                k.dma("sp", kiT[:], kiT_s, writes=[kiT])
                cmask = SB(ph, "cmask", [128, 512], F32)
                k.dma("sp", cmask[:], cmask_d, writes=[cmask])
                qTb = [SB(ph, f"qTb{i}", [128, 8, 128], BF16) for i in range(2)]
                qiTb = [SB(ph, f"qiTb{i}", [64, 8, 128], BF16) for i in range(2)]
                wib = [SB(ph, f"wib{i}", [128, 8], F32) for i in range(2)]
                wabs = SB(ph, "wabs", [128, 8], F32)
                wsgn = SB(ph, "wsgn", [128, 8], F32)
                Rb = [SB(ph, f"Rb{i}", [128, 512], F32) for i in range(4)]
                s2 = SB(ph, "s2", [128, 512], F32)
                s3 = SB(ph, "s3", [128, 512], F32)
                mid = SB(ph, "mid", [128, 1], F32)
                cntb = SB(ph, "cntb", [128, 1], F32)
                cnt2 = SB(ph, "cnt2", [128, 1], F32)
                ajunk = SB(ph, "ajunk", [128, S // 2], BF16)
                tmpb = SB(ph, "tmpb", [128, 1], F32)
                mk = [SB(ph, f"mk{i}", [128, 512], BF16) for i in range(2)]
                mkT = [SB(ph, f"mkT{i}", [128, 512], BF16) for i in range(2)]
                Kc = [SB(ph, f"Kc{i}", [128, 8, 512], BF16) for i in range(3)]
                Vc = [SB(ph, f"Vc{i}", [128, 4, 8, 129], BF16) for i in range(3)]
                Eb = [SB(ph, f"Eb{i}", [128, 512], BF16) for i in range(4)]
                Pb = [SB(ph, f"Pb{i}", [128, 512], BF16) for i in range(4)]
                ysb = SB(ph, "ysb", [128, 1024], BF16)
                rz = SB(ph, "rz", [128, 8], F32)
                yTb = [SB(ph, f"yTb{i}", [128, 8, 128], BF16) for i in range(2)]
                ps_idx = [pb[0], pb[1]]
                ps_s = [pb[0], pb[1], pb[2], pb[3]]
                ps_o = [pb[4], pb[5], pb[6]]
                WSC = (8.0 ** -0.5) * (64.0 ** -0.5)
                ASC = 128.0 ** -0.5
                n_i = 0
                n_c = 0
                for i in range(NO):
                    L = (i + 1) * 512
                    nch = L // 512
                    q_, qi_, w_ = qTb[i % 2], qiTb[i % 2], wib[i % 2]
                    k.dma("sp", q_[:], qT[i], writes=[q_])
                    k.dma("sp", qi_[:], qiT[i], writes=[qi_])
                    k.dma("sp", w_[:], wi_s[i], writes=[w_])
                    k.op("act", lambda: nc.scalar.activation(out=wabs[:], in_=w_[:], func=AF.Abs, scale=WSC),
                         [w_], [wabs])
                    k.op("act", lambda: nc.scalar.activation(out=wsgn[:], in_=w_[:], func=AF.Sign), [w_], [wsgn])
                    for c in range(nch):
                        sl = slice(c * 512, (c + 1) * 512)
                        for h in range(8):
                            ps = ps_idx[n_i % 2]
                            R = Rb[n_i % 4]
                            n_i += 1
                            mm(ps, ps[:, :], qi_[:, h, :], kiT[0:64, sl], True, True, [qi_, kiT])
                            k.op("act", lambda ps=ps, R=R, h=h: nc.scalar.activation(
                                out=R[:], in_=ps[:, :], func=AF.Relu, scale=wabs[:, h:h + 1]), [ps, wabs], [R])
                            if h == 0:
                                k.op("dve", lambda R=R, sl=sl: nc.vector.tensor_scalar(
                                    out=score[:, sl], in0=R[:], scalar1=wsgn[:, 0:1], scalar2=None, op0=OP.mult),
                                    [R, wsgn], [score])
                            elif h < 5:
                                k.op("dve", lambda R=R, sl=sl, h=h: nc.vector.scalar_tensor_tensor(
                                    out=score[:, sl], in0=R[:], scalar=wsgn[:, h:h + 1], in1=score[:, sl],
                                    op0=OP.mult, op1=OP.add), [R, wsgn, score], [score])
                            elif h == 5:
                                k.op("pool", lambda R=R: nc.gpsimd.tensor_scalar(
                                    out=s2[:], in0=R[:], scalar1=wsgn[:, 5:6], scalar2=None, op0=OP.mult),
                                    [R, wsgn], [s2])
                            else:
                                k.op("pool", lambda R=R, h=h: nc.gpsimd.tensor_scalar(
                                    out=s3[:], in0=R[:], scalar1=wsgn[:, h:h + 1], scalar2=None, op0=OP.mult),
                                    [R, wsgn], [s3])
                                k.op("pool", lambda: nc.gpsimd.tensor_tensor(
                                    out=s2[:], in0=s2[:], in1=s3[:], op=OP.add), [s2, s3], [s2])
                                if h == 7:
                                    k.op("dve", lambda sl=sl: nc.vector.tensor_tensor(
                                        out=score[:, sl], in0=score[:, sl], in1=s2[:], op=OP.add),
                                        [score, s2], [score])
                    k.op("dve", lambda: nc.vector.tensor_tensor(out=score[:, L - 512:L], in0=score[:, L - 512:L],
                                                                in1=cmask[:], op=OP.add), [score, cmask], [score])
                    H1 = L // 2
                    n2 = float(L - H1)
                    k.op("dve", lambda: nc.vector.memset(mid[:], 3.14159e-7), [], [mid])
                    for it in range(NBIS):
                        hk = 16.0 * (2.0 ** -it)
                        last = it == NBIS - 1
                        k.op("dve", lambda: nc.vector.tensor_scalar(
                            out=junk[:, 0:1].broadcast_to([128, H1]), in0=score[:, 0:H1], scalar1=mid[:, 0:1],
                            scalar2=0.0, op0=OP.is_ge, op1=OP.add, accum_out=cntb[:, 0:1]), [score, mid],
                            [junk, cntb])
                        k.op("act", lambda: nc.scalar.activation(
                            out=ajunk[:, 0:L - H1], in_=score[:, H1:L], func=AF.Sign, bias=mid[:, 0:1], scale=-1.0,
                            accum_out=cnt2[:, 0:1]), [score, mid], [ajunk, cnt2])
                        k.op("dve", lambda: nc.vector.scalar_tensor_tensor(
                            out=tmpb[:], in0=cntb[:], scalar=2.0, in1=cnt2[:], op0=OP.mult, op1=OP.subtract),
                            [cntb, cnt2], [tmpb])
                        step = (hk / 2.0) if not last else hk
                        mul = 2.0 * step if not last else step
                        k.op("dve", lambda mul=mul: nc.vector.tensor_scalar(
                            out=tmpb[:], in0=tmpb[:], scalar1=511.0 - n2, scalar2=mul, op0=OP.is_ge, op1=OP.mult),
                            [tmpb], [tmpb])
                        k.op("dve", lambda step=step: nc.vector.scalar_tensor_tensor(
                            out=mid[:], in0=tmpb[:], scalar=-step, in1=mid[:], op0=OP.add, op1=OP.add),
                            [tmpb, mid], [mid])
                    for b3 in ps_o:
                        k.op("dve", lambda b3=b3: nc.vector.memset(b3[:, :], 0.0), [], [b3])
                    def load(c):
                        sl = slice(c * 512, (c + 1) * 512)
                        K_, V_ = Kc[c % 3], Vc[c % 3]
                        k.dma("sp", K_[:], KT.rearrange("h p s -> p h s")[:, :, sl], writes=[K_])
                        k.dma("sp", V_[:].rearrange("p b h d -> p b (h d)"),
                              Vs[c * 512:(c + 1) * 512, :].rearrange("(b p) e -> p b e", p=128), writes=[V_])

                    def prep(c):
                        sl = slice(c * 512, (c + 1) * 512)
                        m_, mT_ = mk[c % 2], mkT[c % 2]
                        if c + 1 < nch:
                            load(c + 1)
                        k.op("dve", lambda: nc.vector.tensor_scalar(
                            out=m_[:], in0=score[:, sl], scalar1=mid[:, 0:1], scalar2=None, op0=OP.is_ge),
                            [score, mid], [m_])
                        for b in range(4):
                            transp(ptr, ptr[:, b * 128:(b + 1) * 128], m_[:, b * 128:(b + 1) * 128], ident, [m_])
                        evac(mT_[:], ptr[:, 0:512], [ptr], [mT_])

                    def qk(c, h):
                        ps = ps_s[h % 4]
                        K_ = Kc[c % 3]
                        for b in range(4):
                            mm(ps, ps[:, b * 128:(b + 1) * 128], K_[:, h, b * 128:(b + 1) * 128], q_[:, h, :],
                               True, True, [K_, q_])

                    def rest(c, h):
                        ps = ps_s[h % 4]
                        V_, mT_ = Vc[c % 3], mkT[c % 2]
                        E_, P_ = Eb[h % 4], Pb[h % 4]
                        k.op("act", lambda: nc.scalar.activation(
                            out=E_[:], in_=ps[:, :], func=AF.Exp, scale=ASC), [ps], [E_])
                        k.op("dve", lambda: nc.vector.tensor_tensor(
                            out=P_[:], in0=E_[:], in1=mT_[:], op=OP.mult), [E_, mT_], [P_])
                        po = ps_o[h // 3]
                        oc = (h % 3) * 129
                        for b in range(4):
                            mm_acc(po, po[:, oc:oc + 129], P_[:, b * 128:(b + 1) * 128], V_[:, b, h, :],
                                   [P_, V_], inc=(b == 3))

                    items = [(c, h) for c in range(nch) for h in range(8)]
                    DEPTH = 3
                    prepped = set()

                    def issue_qk(j):
                        if j < len(items):
                            c2, h2 = items[j]
                            if c2 not in prepped:
                                prepped.add(c2)
                                prep(c2)
                            qk(c2, h2)

                    load(0)
                    for j in range(DEPTH):
                        issue_qk(j)
                    for idx, (c, h) in enumerate(items):
                        issue_qk(idx + DEPTH)
                        rest(c, h)
                    for h in range(8):
                        po = ps_o[h // 3]
                        oc = (h % 3) * 129
                        k.op("dve", lambda po=po, oc=oc, h=h: nc.vector.reciprocal(
                            out=rz[:, h:h + 1], in_=po[:, oc + 128:oc + 129]), [po], [rz])
                        k.op("dve", lambda po=po, oc=oc, h=h: nc.vector.tensor_scalar(
                            out=ysb[:, h * 128:(h + 1) * 128], in0=po[:, oc:oc + 128], scalar1=rz[:, h:h + 1],
                            scalar2=None, op0=OP.mult), [po, rz], [ysb])
                    yT_ = yTb[i % 2]
                    for h in range(8):
                        transp(ptr, ptr[:, h * 128:(h + 1) * 128], ysb[:, h * 128:(h + 1) * 128], ident, [ysb],
                               inc=(h == 7))
                    evac(yT_[:], ptr[:].rearrange("p (h t) -> p h t", h=8), [ptr], [yT_])
                    k.dma("pool", yaT[i], yT_[:], reads=[yT_])
            k.barrier()

        if "2b" in phases:
            with ExitStack() as ph:
                stg = [SB(ph, f"c_s{i}", [128, 2048], F32) for i in range(2)]
                wa = SB(ph, "wa", [128, 8, D], BF16)
                wb = SB(ph, "wb", [128, 8, D], BF16)
                load_w(stg, wa, 0, w_ba, 2048)
                load_w(stg, wb, 0, w_bg, 2048)
                wsT = SB(ph, "wsT", [128, 8, 128], BF16)
                trilb = SB(ph, "trilb", [128, 128], F32)
                k.dma("sp", trilb[:], tril_d, writes=[trilb])
                for g in range(8):
                    sg = stg[g % 2]
                    k.dma("sp", sg[:, 0:128], wsT_d[g], writes=[sg])
                    k.op("dve", lambda g=g, sg=sg: nc.vector.tensor_tensor(out=wsT[:, g, :], in0=sg[:, 0:128],
                                                                           in1=trilb[:], op=OP.mult),
                         [sg, trilb], [wsT])
                bT = SB(ph, "bT", [128, 8], F32)
                k.dma("sp", bT[:], bT_d, writes=[bT])
                ggm = SB(ph, "ggm", [128, 1024], F32)
                k.dma("sp", ggm[:], g_gm_d, writes=[ggm])
                gub = [SB(ph, f"gub{i}", [128, 1024], BF16) for i in range(2)]
                gvb = [SB(ph, f"gvb{i}", [128, 1024], BF16) for i in range(2)]
                sab = [SB(ph, f"sab{i}", [128, D], BF16) for i in range(2)]
                sbb = [SB(ph, f"sbb{i}", [128, D], BF16) for i in range(2)]
                yab = [SB(ph, f"yab{i}", [128, 8, 128], BF16) for i in range(2)]
                vsq = SB(ph, "vsq", [128, 1024], BF16)
                vss = SB(ph, "vss", [128, 1], F32)
                vrs = SB(ph, "vrs", [128, 1], F32)
                vn = SB(ph, "vn", [128, 1024], BF16)
                yb = SB(ph, "yb", [128, 1024], BF16)
                ybT = SB(ph, "ybT", [128, 8, 128], BF16)
                mixa = SB(ph, "mixa", [128, D], F32)
                mixc = SB(ph, "mixc", [128, D], F32)
                mixb = SB(ph, "mixb", [128, D], BF16)
                mixTb = [SB(ph, f"mixT{i}", [128, KC, 128], BF16) for i in range(2)]
                n_e = 0
                for i in range(NO):
                    gu_, gv_, sa_, sb_, ya_ = gub[i % 2], gvb[i % 2], sab[i % 2], sbb[i % 2], yab[i % 2]
                    mixT = mixTb[i % 2]
                    k.dma("sp", gu_[:], gu_s[i], writes=[gu_])
                    k.dma("sp", gv_[:], gv_s[i], writes=[gv_])
                    k.dma("sp", sa_[:], sga_s[i], writes=[sa_])
                    k.dma("sp", sb_[:], sgb_s[i], writes=[sb_])
                    k.dma("sp", ya_[:], yaT[i], writes=[ya_])
                    k.op("act", lambda gv_=gv_: nc.scalar.activation(out=vsq[:], in_=gv_[:], func=AF.Square,
                                                                    accum_out=vss[:, 0:1]), [gv_], [vsq, vss])
                    k.op("dve", lambda: nc.vector.tensor_scalar(out=vrs[:], in0=vss[:, 0:1], scalar1=1.0 / 1024,
                                                                scalar2=EPS, op0=OP.mult, op1=OP.add), [vss], [vrs])
                    k.op("act", lambda: nc.scalar.activation(out=vrs[:], in_=vrs[:], func=AF.Sqrt), [vrs], [vrs])
                    k.op("dve", lambda: nc.vector.reciprocal(out=vrs[:], in_=vrs[:]), [vrs], [vrs])
                    k.op("dve", lambda gv_=gv_: nc.vector.scalar_tensor_tensor(
                        out=vn[:], in0=gv_[:], scalar=vrs[:, 0:1], in1=ggm[:], op0=OP.mult, op1=OP.mult),
                        [gv_, vrs, ggm], [vn])
                    pz = [pb[0], pb[1]]
                    for g in range(8):
                        ps = pz[g // 4]
                        mm(ps, ps[:, (g % 4) * 128:(g % 4 + 1) * 128], wsT[:, g, :], vn[:, g * 128:(g + 1) * 128],
                           True, True, [wsT, vn], inc=(g % 4 == 3))
                    for g in range(8):
                        ps = pz[g // 4]
                        k.op("dve", lambda ps=ps, g=g, gu_=gu_: nc.vector.scalar_tensor_tensor(
                            out=yb[:, g * 128:(g + 1) * 128], in0=ps[:, (g % 4) * 128:(g % 4 + 1) * 128],
                            scalar=bT[:, g:g + 1], in1=gu_[:, g * 128:(g + 1) * 128], op0=OP.add, op1=OP.mult),
                            [ps, bT, gu_], [yb])
                    for g in range(8):
                        transp(ptr, ptr[:, g * 128:(g + 1) * 128], yb[:, g * 128:(g + 1) * 128], ident, [yb],
                               inc=(g == 7))
                    evac(ybT[:], ptr[:].rearrange("p (g t) -> p g t", g=8), [ptr], [ybT])
                    for cc in range(4):
                        csl = slice(cc * 512, (cc + 1) * 512)
                        psa = pb[2 + (n_e % 2)]
                        psb = pb[4 + (n_e % 2)]
                        n_e += 1
                        for kc in range(8):
                            mm(psa, psa[:, :], ya_[:, kc, :], wa[:, kc, csl], kc == 0, kc == 7, [ya_, wa])
                        for kc in range(8):
                            mm(psb, psb[:, :], ybT[:, kc, :], wb[:, kc, csl], kc == 0, kc == 7, [ybT, wb])
                        k.op("dve", lambda psa=psa, csl=csl, sa_=sa_: nc.vector.tensor_tensor(
                            out=mixa[:, csl], in0=psa[:, :], in1=sa_[:, csl], op=OP.mult), [psa, sa_], [mixa])
                        k.op("dve", lambda psb=psb, csl=csl, sb_=sb_: nc.vector.tensor_tensor(
                            out=mixc[:, csl], in0=psb[:, :], in1=sb_[:, csl], op=OP.mult), [psb, sb_], [mixc])
                        k.op("pool", lambda csl=csl: nc.gpsimd.tensor_tensor(
                            out=mixb[:, csl], in0=mixc[:, csl], in1=mixa[:, csl], op=OP.add), [mixa, mixc], [mixb])
                    for half in range(2):
                        for j in range(8):
                            kc = half * 8 + j
                            transp(ptr, ptr[:, j * 128:(j + 1) * 128], mixb[:, kc * 128:(kc + 1) * 128], ident,
                                   [mixb], inc=(j == 7))
                        evac(mixT[:, half * 8:half * 8 + 8, :], ptr[:].rearrange("p (j t) -> p j t", j=8), [ptr],
                             [mixT])
                    k.dma("pool", mixT_s[i], mixT[:], reads=[mixT])
            k.barrier()

        if "2c" in phases:
            with ExitStack() as ph:
                stg = [SB(ph, f"c2_s{i}", [128, 2048], F32) for i in range(2)]
                wo = SB(ph, "wo", [128, KC, D], BF16)
                load_w(stg, wo, 0, w_o, 2048)
                xob = [SB(ph, f"xob{i}", [128, D], F32) for i in range(2)]
                mixTb = [SB(ph, f"c2_mT{i}", [128, KC, 128], BF16) for i in range(2)]
                n_e = 0
                for i in range(NO):
                    xo_, mixT = xob[i % 2], mixTb[i % 2]
                    k.dma("sp", xo_[:], x_own[i * 128:(i + 1) * 128, :], writes=[xo_])
                    k.dma("sp", mixT[:], mixT_s[i], writes=[mixT])
                    for cc in range(4):
                        csl = slice(cc * 512, (cc + 1) * 512)
                        ps = pb[n_e % 4]
                        n_e += 1
                        for kc in range(KC):
                            mm(ps, ps[:, :], mixT[:, kc, :], wo[:, kc, csl], kc == 0, kc == KC - 1, [mixT, wo])
                        k.op("dve", lambda ps=ps, csl=csl, xo_=xo_: nc.vector.tensor_tensor(
                            out=xo_[:, csl], in0=ps[:, :], in1=xo_[:, csl], op=OP.add), [ps, xo_], [xo_])
                    k.dma("pool", x1_s[i], xo_[:], reads=[xo_])
            k.barrier()

        if "3a" in phases:
            with ExitStack() as ph:
                stg = [SB(ph, f"d_s{i}", [128, 2048], F32) for i in range(2)]
                wq = SB(ph, "wq", [128, KC, D], BF16)
                load_w(stg, wq, 0, w_pq, 2048)
                skT = SB(ph, "skT", [128, 16, 128], BF16)
                for b in range(16):
                    sg = stg[b % 2]
                    k.dma("sp", sg[:, 0:128], skT_d[b], writes=[sg])
                    cast("dve", skT[:, b, :], sg[:, 0:128], [sg], [skT])
                gbc = SB(ph, "gbc3", [128, D], F32)
                k.dma("sp", gbc[:], g_ffn_d, writes=[gbc])
                xin = [SB(ph, f"d_x{i}", [128, D], F32) for i in range(2)]
                sqj = SB(ph, "d_sqj", [128, D], BF16)
                hn = SB(ph, "d_hn", [128, D], BF16)
                ssq = SB(ph, "d_ssq", [128, 1], F32)
                rstd = SB(ph, "d_rstd", [128, 1], F32)
                hTbs = [SB(ph, f"d_hT{i}", [128, KC, 128], BF16) for i in range(2)]
                qpT = SB(ph, "qpT", [128, 16, 128], BF16)
                ssbs = [SB(ph, f"ssb{i}", [128, 16, 128], F32) for i in range(2)]
                n_e = 0
                for i in range(NO):
                    xt = xin[i % 2]
                    hTb = hTbs[i % 2]
                    ssb = ssbs[i % 2]
                    k.dma("sp", xt[:], x1_s[i], writes=[xt])
                    rms_rows(ph, xt, ssq, rstd, sqj)
                    norm_transpose(xt, gbc, rstd, hn, hTb, lambda k0, n, hTb=hTb: hTb[:, k0:k0 + n, :])
                    k.dma("pool", hn2T[i], hTb[:], reads=[hTb])
                    for bq in range(4):
                        ps = pb[n_e % 2]
                        n_e += 1
                        for bb in range(4):
                            blk = bq * 4 + bb
                            for kc in range(KC):
                                mm(ps, ps[:, bb * 128:(bb + 1) * 128], wq[:, kc, blk * 128:(blk + 1) * 128],
                                   hTb[:, kc, :], kc == 0, kc == KC - 1, [wq, hTb], inc=(kc == KC - 1 and bb == 3))
                        evac(qpT[:, bq * 4:bq * 4 + 4, :], ps[:, :].rearrange("p (b t) -> p b t", b=4), [ps], [qpT])
                    for bq in range(4):
                        ps = pb[2 + (n_e % 2)]
                        n_e += 1
                        for bb in range(4):
                            blk = bq * 4 + bb
                            mm(ps, ps[:, bb * 128:(bb + 1) * 128], qpT[:, blk, :], skT[:, blk, :], True, True,
                               [qpT, skT], inc=(bb == 3))
                        k.op("act", lambda ps=ps, bq=bq, ssb=ssb: nc.scalar.copy(
                            out=ssb[:, bq * 4:bq * 4 + 4, :], in_=ps[:, :].rearrange("p (b n) -> p b n", b=4)),
                            [ps], [ssb])
                    k.dma("pool", s_s[i], ssb[:], reads=[ssb])
            k.barrier()

        if "3b" in phases:
            with ExitStack() as ph:
                iota_i = SB(ph, "iota_i", [128, 128], F32)
                k.dma("sp", iota_i[:], iota_d, writes=[iota_i])
                ssb = SB(ph, "ssb", [128, 16, 128], F32)
                top = SB(ph, "top", [128, 16, 16], F32)
                tix = SB(ph, "tix", [128, 16, 16], U32)
                tixf = SB(ph, "tixf", [128, 16, 16], F32)
                cand = SB(ph, "cand", [128, 8, 256], F32)
                best = SB(ph, "best", [128, 8, 16], F32)
                bix = SB(ph, "bix", [128, 8, 16], U32)
                ba_u = SB(ph, "ba_u", [128, 8, 16], U32)
                bb_u = SB(ph, "bb_u", [128, 8, 16], U32)
                ba_f = SB(ph, "ba_f", [128, 8, 16], F32)
                bb_f = SB(ph, "bb_f", [128, 8, 16], F32)
                eq = SB(ph, "eq", [128, 8, 16, 16], F32)
                isel = SB(ph, "isel", [128, 8, 16], F32)
                jsel = SB(ph, "jsel", [128, 8, 16], F32)
                gate = SB(ph, "gate", [128, 8, 16], F32)
                gsum = SB(ph, "gsum", [128, 8], F32)
                iT = SB(ph, "iT", [128, 128], F32)
                jT = SB(ph, "jT", [128, 128], F32)
                gT = SB(ph, "gT", [128, 128], F32)
                A_all = SB(ph, "A_all", [128, 128, 128], BF16)
                B_all = SB(ph, "B_all", [128, 128, 128], BF16)
                Gt = [SB(ph, f"Gt{i}", [128, 128, 128], BF16) for i in range(1)]
                for i in range(NO):
                    k.dma("sp", ssb[:], s_s[i], writes=[ssb])
                    for blk in range(16):
                        k.op("dve", lambda blk=blk: nc.vector.max(out=top[:, blk, 0:8], in_=ssb[:, blk, :]),
                             [ssb], [top])
                        k.op("dve", lambda blk=blk: nc.vector.max_index(out=tix[:, blk, 0:8], in_max=top[:, blk, 0:8],
                                                                        in_values=ssb[:, blk, :]), [ssb, top], [tix])
                        k.op("dve", lambda blk=blk: nc.vector.match_replace(
                            out=ssb[:, blk, :], in_to_replace=top[:, blk, 0:8], in_values=ssb[:, blk, :],
                            imm_value=NEG), [ssb, top], [ssb])
                        k.op("dve", lambda blk=blk: nc.vector.max(out=top[:, blk, 8:16], in_=ssb[:, blk, :]),
                             [ssb], [top])
                        k.op("dve", lambda blk=blk: nc.vector.max_index(out=tix[:, blk, 8:16],
                                                                        in_max=top[:, blk, 8:16],
                                                                        in_values=ssb[:, blk, :]),
                             [ssb, top], [tix])
                    k.op("dve", lambda: nc.vector.tensor_copy(out=tixf[:], in_=tix[:]), [tix], [tixf])
                    topv = top[:].rearrange("p (h two) k -> p h two k", two=2)
                    k.op("dve", lambda: nc.vector.tensor_tensor(
                        out=cand[:].rearrange("p h (a b) -> p h a b", a=16),
                        in0=topv[:, :, 0, :].unsqueeze(3).broadcast_to([128, 8, 16, 16]),
                        in1=topv[:, :, 1, :].unsqueeze(2).broadcast_to([128, 8, 16, 16]), op=OP.add), [top], [cand])
                    for h in range(8):
                        k.op("dve", lambda h=h: nc.vector.max(out=best[:, h, 0:8], in_=cand[:, h, :]), [cand], [best])
                        k.op("dve", lambda h=h: nc.vector.max_index(out=bix[:, h, 0:8], in_max=best[:, h, 0:8],
                                                                    in_values=cand[:, h, :]), [cand, best], [bix])
                        k.op("dve", lambda h=h: nc.vector.match_replace(
                            out=cand[:, h, :], in_to_replace=best[:, h, 0:8], in_values=cand[:, h, :],
                            imm_value=NEG), [cand, best], [cand])
                        k.op("dve", lambda h=h: nc.vector.max(out=best[:, h, 8:16], in_=cand[:, h, :]),
                             [cand], [best])
                        k.op("dve", lambda h=h: nc.vector.max_index(out=bix[:, h, 8:16], in_max=best[:, h, 8:16],
                                                                    in_values=cand[:, h, :]), [cand, best], [bix])
                    k.op("dve", lambda: nc.vector.tensor_single_scalar(out=ba_u[:], in_=bix[:], scalar=4,
                                                                       op=OP.logical_shift_right), [bix], [ba_u])
                    k.op("dve", lambda: nc.vector.tensor_single_scalar(out=bb_u[:], in_=bix[:], scalar=15,
                                                                       op=OP.bitwise_and), [bix], [bb_u])
                    k.op("dve", lambda: nc.vector.tensor_copy(out=ba_f[:], in_=ba_u[:]), [ba_u], [ba_f])
                    k.op("dve", lambda: nc.vector.tensor_copy(out=bb_f[:], in_=bb_u[:]), [bb_u], [bb_f])
                    tixv = tixf[:].rearrange("p (h two) k -> p h two k", two=2)
                    iota16 = iota_i[:, 0:16].unsqueeze(1).unsqueeze(1).broadcast_to([128, 8, 16, 16])
                    for (sel_f, which, dst) in ((ba_f, 0, isel), (bb_f, 1, jsel)):
                        k.op("dve", lambda sel_f=sel_f: nc.vector.tensor_tensor(
                            out=eq[:], in0=sel_f[:].unsqueeze(3).broadcast_to([128, 8, 16, 16]), in1=iota16,
                            op=OP.is_equal), [sel_f, iota_i], [eq])
                        k.op("dve", lambda which=which: nc.vector.tensor_tensor(
                            out=eq[:], in0=eq[:], in1=tixv[:, :, which, :].unsqueeze(2).broadcast_to([128, 8, 16, 16]),
                            op=OP.mult), [eq, tixf], [eq])
                        k.op("dve", lambda dst=dst: nc.vector.tensor_reduce(out=dst[:], in_=eq[:], axis=AX.X,
                                                                            op=OP.add), [eq], [dst])
                    k.op("dve", lambda: nc.vector.tensor_tensor(
                        out=gate[:], in0=best[:], in1=best[:, :, 0:1].broadcast_to([128, 8, 16]), op=OP.subtract),
                        [best], [gate])
                    k.op("act", lambda: nc.scalar.activation(out=gate[:], in_=gate[:], func=AF.Exp), [gate], [gate])
                    k.op("dve", lambda: nc.vector.tensor_reduce(out=gsum[:], in_=gate[:], axis=AX.X, op=OP.add),
                         [gate], [gsum])
                    k.op("dve", lambda: nc.vector.reciprocal(out=gsum[:], in_=gsum[:]), [gsum], [gsum])
                    k.op("dve", lambda: nc.vector.tensor_tensor(
                        out=gate[:], in0=gate[:], in1=gsum[:].unsqueeze(2).broadcast_to([128, 8, 16]), op=OP.mult),
                        [gate, gsum], [gate])
                    pt_f = pb[4]
                    for (src, dst, col) in ((isel, iT, 0), (jsel, jT, 1), (gate, gT, 2)):
                        k.op("pe", lambda src=src, col=col: nc.tensor.transpose(
                            out=pt_f[:, col * 128:(col + 1) * 128], in_=src[:].rearrange("p h k -> p (h k)"),
                            identity=identf[:]), [src, identf], [pt_f], skip_self=True)
                        evac(dst[:], pt_f[:, col * 128:(col + 1) * 128], [pt_f], [dst])
                    iota_b = iota_i[:].unsqueeze(1).broadcast_to([128, 128, 128])
                    k.op("dve", lambda: nc.vector.tensor_tensor(
                        out=A_all[:], in0=iota_b, in1=iT[:].unsqueeze(2).broadcast_to([128, 128, 128]),
                        op=OP.is_equal), [iota_i, iT], [A_all])
                    k.op("pool", lambda: nc.gpsimd.tensor_tensor(
                        out=A_all[:], in0=A_all[:], in1=gT[:].unsqueeze(2).broadcast_to([128, 128, 128]),
                        op=OP.mult), [A_all, gT], [A_all])
                    k.op("dve", lambda: nc.vector.tensor_tensor(
                        out=B_all[:], in0=iota_b, in1=jT[:].unsqueeze(2).broadcast_to([128, 128, 128]),
                        op=OP.is_equal), [iota_i, jT], [B_all])
                    G_ = Gt[0]
                    for t4 in range(32):
                        ps = pb[5 + (t4 % 2)]
                        for tt in range(4):
                            t = t4 * 4 + tt
                            mm(ps, ps[:, tt * 128:(tt + 1) * 128], B_all[:, t, :], A_all[:, t, :], True, True,
                               [A_all, B_all], inc=(tt == 3))
                        k.op("act", lambda ps=ps, t4=t4: nc.scalar.copy(
                            out=G_[:, :, t4 * 4:t4 * 4 + 4].rearrange("p i t -> p t i"),
                            in_=ps[:, :].rearrange("p (t i) -> p t i", t=4)), [ps], [G_])
                    for q4 in range(4):
                        k.dma("pool", G_s[i][:, q4 * 32:(q4 + 1) * 32, :], G_[:, q4 * 32:(q4 + 1) * 32, :], reads=[G_])
            k.barrier()

        if "4" in phases:
            TT = min(4, NO)
            NTT = NO // TT
            TW = TT * 128
            GB = 4
            with ExitStack() as ph:
                gfin = SB(ph, "gfin", [128, D], F32)
                k.dma("sp", gfin[:], g_fin_d, writes=[gfin])
                hT4 = SB(ph, "hT4", [128, KC, TW], BF16)
                acc = SB(ph, "acc", [128, TT, D], F32)
                Ug = [SB(ph, f"Ug{i}", [128, KC, GB * 128], BF16) for i in range(2)]
                Vg = [SB(ph, f"Vg{i}", [128, GB, D], BF16) for i in range(2)]
                Gg = [SB(ph, f"Gg{i}", [128, GB, TW], BF16) for i in range(2)]
                GA = [SB(ph, f"GA{i}", [128, GB, TW], BF16) for i in range(2)]
                gl = [SB(ph, f"gl{i}", [128, TW], BF16) for i in range(2)]
                sqj = SB(ph, "e_sqj", [128, D], BF16)
                ssq = SB(ph, "e_ssq", [128, 1], F32)
                rstd = SB(ph, "e_rstd", [128, 1], F32)
                ob = [SB(ph, f"e_ob{i}", [128, D], F32) for i in range(2)]
                n_g = 0
                n_e = 0
                for tt_i in range(NTT):
                    for s_ in range(TT):
                        i = tt_i * TT + s_
                        k.dma("sp", hT4[:, :, s_ * 128:(s_ + 1) * 128], hn2T[i], writes=[hT4])
                        k.dma("sp", acc[:, s_, :], x1_s[i], writes=[acc])
                    for g in range(128 // GB):
                        U_, V_, G_, A_ = Ug[n_g % 2], Vg[n_g % 2], Gg[n_g % 2], GA[n_g % 2]
                        n_g += 1
                        e0 = g * GB * 128
                        k.dma("sp", U_[:], Ubf[:, e0:e0 + GB * 128].rearrange("(kc p) e -> p kc e", p=128),
                              writes=[U_])
                        k.dma("sp", V_[:], Vbf[e0:e0 + GB * 128, :].rearrange("(b p) d -> p b d", p=128),
                              writes=[V_])
                        for s_ in range(TT):
                            i = tt_i * TT + s_
                            k.dma("sp", G_[:, :, s_ * 128:(s_ + 1) * 128], G_s[i][:, g * GB:(g + 1) * GB, :],
                                  writes=[G_])
                        for b in range(GB):
                            ps = pb[n_e % 2]
                            n_e += 1
                            for kc in range(KC):
                                mm(ps, ps[:, 0:TW], U_[:, kc, b * 128:(b + 1) * 128], hT4[:, kc, :], kc == 0,
                                   kc == KC - 1, [U_, hT4])
                            g_ = gl[b % 2]
                            k.op("act", lambda ps=ps, g_=g_: nc.scalar.activation(
                                out=g_[:], in_=ps[:, 0:TW], func=AF.Gelu_apprx_tanh), [ps], [g_])
                            k.op("dve", lambda g_=g_, b=b, A_=A_, G_=G_: nc.vector.tensor_tensor(
                                out=A_[:, b, :], in0=g_[:], in1=G_[:, b, :], op=OP.mult), [g_, G_], [A_])
                        for s_ in range(TT):
                            for cc in range(4):
                                csl = slice(cc * 512, (cc + 1) * 512)
                                ps = pb[2 + (n_e % 4)]
                                n_e += 1
                                for b in range(GB):
                                    mm(ps, ps[:, :], A_[:, b, s_ * 128:(s_ + 1) * 128], V_[:, b, csl], b == 0,
                                       b == GB - 1, [A_, V_])
                                k.op("dve", lambda ps=ps, s_=s_, csl=csl: nc.vector.tensor_tensor(
                                    out=acc[:, s_, csl], in0=ps[:, :], in1=acc[:, s_, csl], op=OP.add),
                                    [ps, acc], [acc])
                    for s_ in range(TT):
                        i = tt_i * TT + s_
                        o_ = ob[s_ % 2]
                        k.op("act", lambda s_=s_: nc.scalar.activation(out=sqj[:], in_=acc[:, s_, :], func=AF.Square,
                                                                       accum_out=ssq[:, 0:1]), [acc], [sqj, ssq])
                        k.op("dve", lambda: nc.vector.tensor_scalar(out=rstd[:], in0=ssq[:, 0:1], scalar1=1.0 / D,
                                                                    scalar2=EPS, op0=OP.mult, op1=OP.add),
                             [ssq], [rstd])
                        k.op("act", lambda: nc.scalar.activation(out=rstd[:], in_=rstd[:], func=AF.Sqrt),
                             [rstd], [rstd])
                        k.op("dve", lambda: nc.vector.reciprocal(out=rstd[:], in_=rstd[:]), [rstd], [rstd])
                        k.op("dve", lambda s_=s_, o_=o_: nc.vector.scalar_tensor_tensor(
                            out=o_[:], in0=acc[:, s_, :], scalar=rstd[:, 0:1], in1=gfin[:], op0=OP.mult, op1=OP.mult),
                            [acc, rstd, gfin], [o_])
                        k.dma("pool", out_d[i * 128:(i + 1) * 128, :], o_[:], reads=[o_])
            k.barrier()
        else:
            pass
        k.barrier()
        nc._kb_inst = k.n_inst
        nc._kb_want = k.want
    return nc


def build2(S, phases=None, expose=()):
    dry = build(S, phases, expose, None)
    return build(S, phases, expose, dry._kb_want)


def _host_inputs(inp, S, n_cores=8, phases=None):
    x = np.asarray(inp["x"], np.float32)
    B = x.shape[0]
    NT = S // 128
    NO = NT // 4
    f = lambda a: np.ascontiguousarray(np.asarray(a, np.float32))
    bc = lambda v, n: f(np.broadcast_to(np.asarray(v, np.float32).reshape(1, n), (128, n)))
    common = {
        "tril": f(np.triu(np.ones((128, 128), np.float32))),
        "ident": f(np.eye(128, dtype=np.float32)),
        "iota": f(np.broadcast_to(np.arange(128, dtype=np.float32)[None, :], (128, 128))),
        "g_mix": bc(inp["ln_mix_g"][0], D),
        "g_ffn": bc(inp["ln_ffn_g"][0], D),
        "g_fin": bc(inp["ln_final_g"], D),
        "g_gm": bc(inp["gmlp_norm_g"][0], 1024),
        "w_in": f(inp["w_in"][0]),
        "wsT": f(np.transpose(inp["w_spatial"][0], (0, 2, 1))),
        "bT": f(np.transpose(inp["b_spatial"][0], (1, 0))),
        "w_ba": f(inp["w_branch_attn"][0]),
        "w_bg": f(inp["w_branch_gmlp"][0]),
        "w_o": f(inp["w_out"][0]),
        "w_pq": f(inp["peer_w_q"][0]),
        "skT": f(np.transpose(np.asarray(inp["peer_sub_keys"][0]).reshape(16, 128, 128), (0, 2, 1))),
    }
    if phases is None or "0" in phases:
        common["uT"] = f(np.asarray(inp["peer_u"][0]).T)
        common["pv"] = f(inp["peer_v"][0])
    maps = []
    for c in range(n_cores):
        b, r = c // 4, c % 4
        xb = x[b % B, :S]
        own = np.concatenate([xb[(4 * i + r) * 128:(4 * i + r + 1) * 128] for i in range(NO)], axis=0)
        sp = np.arange(512)[None, :]
        tq = np.arange(128)[:, None]
        cm = np.where(sp <= r * 128 + tq, 0.0, NEG).astype(np.float32)
        m = dict(common)
        m["x_all"] = f(xb)
        m["x_own"] = f(own)
        m["cmask"] = f(cm)
        maps.append(m)
    return maps


def _gather(results, S, B=2):
    NT = S // 128
    NO = NT // 4
    out = np.zeros((B, S, D), np.float32)
    for c, rmap in enumerate(results):
        b, r = c // 4, c % 4
        o = np.asarray(rmap["out"])
        for i in range(NO):
            out[b, (4 * i + r) * 128:(4 * i + r + 1) * 128] = o[i * 128:(i + 1) * 128]
    return out


def kernel(**inputs):
    S = inputs["x"].shape[1]
    nc = build2(S)
    maps = _host_inputs(inputs, S)
    res = run_bass_kernel_spmd(nc, maps, core_ids=list(range(8)))
    return _gather(res.results, S)
```

```python
from contextlib import ExitStack
import numpy as np
import concourse.bass as bass
import concourse.mybir as mybir
from concourse.bass_utils import run_bass_kernel_spmd

F32 = mybir.dt.float32
BF16 = mybir.dt.bfloat16
U32 = mybir.dt.uint32
AF = mybir.ActivationFunctionType
OP = mybir.AluOpType
AX = mybir.AxisListType

D = 2048
KC = 16
NE = 16384
EPS = 1e-6
NEG = -1.0e30
C_Q, C_K, C_V, C_QI, C_KI, C_WI, C_GU, C_GV, C_GA, C_GB = 0, 1024, 2048, 3072, 3584, 3648, 3656, 4680, 5704, 7752
NBIS = 21


class Dep:
    __slots__ = ("w", "r")

    def __init__(self):
        self.w = None
        self.r = []


class Buf:
    def __init__(self, t):
        self.t = t
        self.d = Dep()

    def __getitem__(self, idx):
        return self.t[idx]


class KB:
    def __init__(self, nc, stack, needed=None, n_dma_sems=20):
        self.nc = nc
        self.dry = needed is None
        self.engs = {"pe": nc.tensor, "act": nc.scalar, "dve": nc.vector,
                     "pool": nc.gpsimd, "sp": nc.sync}
        self.sem = {}
        self.cnt = {}
        for e in self.engs:
            self.sem[e] = stack.enter_context(nc.semaphore("s_" + e))
            self.cnt[e] = 0
        self.dsem = {}
        self.dnext = {}
        for q in ("sp", "pool", "act"):
            lst = []
            for i in range(n_dma_sems):
                key = f"d_{q}_{i}"
                self.sem[key] = stack.enter_context(nc.semaphore(key))
                self.cnt[key] = 0
                lst.append(key)
            self.dsem[q] = lst
            self.dnext[q] = 0
        self.seen = {e: {} for e in self.engs}
        self.snap = {}
        self.want = {e: set() for e in self.engs}
        if needed is not None:
            self.rank = {e: {n: i + 1 for i, n in enumerate(sorted(needed[e]))} for e in self.engs}
        self.n_inst = 0

    def _wait(self, e, key, val):
        if val <= 0 or self.seen[e].get(key, 0) >= val:
            return
        se = self.seen[e]
        se[key] = val
        sn = self.snap.get((key, val))
        if sn:
            for kk, vv in sn.items():
                if se.get(kk, 0) < vv:
                    se[kk] = vv
        self.n_inst += 1
        if key in self.engs:
            if self.dry:
                self.want[key].add(val)
            else:
                self.engs[e].wait_ge(self.sem[key], self.rank[key][val])
        elif not self.dry:
            self.engs[e].wait_ge(self.sem[key], val)

    def _deps(self, e, reads, writes, skip_self):
        need = {}

        def add(tok):
            if skip_self and tok[0] == e:
                return
            if need.get(tok[0], 0) < tok[1]:
                need[tok[0]] = tok[1]

        for b in reads:
            if b.d.w is not None:
                add(b.d.w)
        for b in writes:
            d = b.d
            if d.w is not None:
                add(d.w)
            for rr in d.r:
                add(rr)
        for key, val in need.items():
            self._wait(e, key, val)

    def _mark(self, tok, reads, writes):
        for b in reads:
            d = b.d
            d.r.append(tok)
            if len(d.r) > 48:
                mx = {}
                for kk, v in d.r:
                    if mx.get(kk, 0) < v:
                        mx[kk] = v
                d.r = list(mx.items())
        for b in writes:
            b.d.w = tok
            b.d.r = []

    def op(self, e, fn, reads=(), writes=(), inc=True, skip_self=False):
        self._deps(e, reads, writes, skip_self)
        self.cnt[e] += 1
        n = self.cnt[e]
        self.n_inst += 1
        if not self.dry:
            ins = fn()
            if n in self.rank[e]:
                ins.then_inc(self.sem[e], 1)
        tok = (e, n)
        sn = dict(self.seen[e])
        sn[e] = n - 1
        self.snap[tok] = sn
        self._mark(tok, reads, writes)

    def dma(self, q, out, in_, reads=(), writes=()):
        lst = self.dsem[q]
        key = lst[self.dnext[q] % len(lst)]
        self.dnext[q] += 1
        self._wait(q, key, self.cnt[key])
        self._deps(q, reads, writes, False)
        self.cnt[key] += 16
        if not self.dry:
            ins = self.engs[q].dma_start(out=out, in_=in_)
            ins.then_inc(self.sem[key], 16)
        tok = (key, self.cnt[key])
        self.snap[tok] = dict(self.seen[q])
        self._mark(tok, reads, writes)
        self.n_inst += 1

    def barrier(self):
        for e in self.engs:
            for key in self.sem:
                if key != e:
                    self._wait(e, key, self.cnt[key])
        self.snap = {}


def build(S, phases=None, expose=(), needed=None):
    if phases is None:
        phases = {"0", "1a", "1b", "2a", "2b", "2c", "3a", "3b", "4"}
    NT = S // 128
    NO = NT // 4
    NST = S // 512
    nc = bass.Bass("TRN2", target_bir_lowering=False)

    def din(name, shape, dt=F32):
        return nc.dram_tensor(name, shape, dt, kind="ExternalInput").ap()

    def dsc(name, shape, dt):
        kind = "ExternalOutput" if name in expose else "Internal"
        return nc.dram_tensor(name, shape, dt, kind=kind).ap()

    x_all = din("x_all", [S, D])
    x_own = din("x_own", [NO * 128, D])
    cmask_d = din("cmask", [128, 512])
    tril_d = din("tril", [128, 128])
    ident_d = din("ident", [128, 128])
    iota_d = din("iota", [128, 128])
    g_mix_d = din("g_mix", [128, D])
    g_ffn_d = din("g_ffn", [128, D])
    g_fin_d = din("g_fin", [128, D])
    g_gm_d = din("g_gm", [128, 1024])
    w_in = din("w_in", [D, 9800])
    wsT_d = din("wsT", [8, 128, 128])
    bT_d = din("bT", [128, 8])
    w_ba = din("w_ba", [1024, D])
    w_bg = din("w_bg", [1024, D])
    w_o = din("w_o", [D, D])
    w_pq = din("w_pq", [D, D])
    skT_d = din("skT", [16, 128, 128])
    if "0" in phases:
        uT_d = din("uT", [D, NE])
        v_d = din("pv", [NE, D])
    out_d = nc.dram_tensor("out", [NO * 128, D], F32, kind="ExternalOutput").ap()

    Ubf = dsc("Ubf", [D, NE], BF16)
    Vbf = dsc("Vbf", [NE, D], BF16)
    KT = dsc("KT", [8, 128, S], BF16)
    Vs = dsc("Vs", [S, 8 * 129], BF16)
    hTo = dsc("hTo", [NO, 128, KC, 128], BF16)
    qT = dsc("qT", [NO, 128, 8, 128], BF16)
    qiT = dsc("qiT", [NO, 64, 8, 128], BF16)
    wi_s = dsc("wi_s", [NO, 128, 8], F32)
    gu_s = dsc("gu_s", [NO, 128, 1024], BF16)
    gv_s = dsc("gv_s", [NO, 128, 1024], BF16)
    sga_s = dsc("sga_s", [NO, 128, D], BF16)
    sgb_s = dsc("sgb_s", [NO, 128, D], BF16)
    yaT = dsc("yaT", [NO, 128, 8, 128], BF16)
    x1_s = dsc("x1_s", [NO, 128, D], F32)
    hn2T = dsc("hn2T", [NO, 128, KC, 128], BF16)
    G_s = dsc("G_s", [NO, 128, 128, 128], BF16)
    kiT_s = dsc("kiT_s", [64, S], BF16)
    mixT_s = dsc("mixT_s", [NO, 128, KC, 128], BF16)
    s_s = dsc("s_s", [NO, 128, 16, 128], F32)

    with ExitStack() as top:
        k = KB(nc, top, needed)

        def SB(st, name, shape, dt):
            return Buf(st.enter_context(nc.sbuf_tensor("sb_" + name, shape, dt)))

        def PS(st, name, shape, dt):
            return Buf(st.enter_context(nc.psum_tensor("ps_" + name, shape, dt)))

        pb = [PS(top, f"pb{i}", [128, 512], F32) for i in range(7)]
        ptr = PS(top, "ptr", [128, 1024], BF16)

        ident = SB(top, "ident", [128, 128], BF16)
        identf = SB(top, "identf", [128, 128], F32)
        junk = SB(top, "junk", [128, 8], BF16)
        k.dma("sp", identf[:], ident_d, writes=[identf])
        k.op("dve", lambda: nc.vector.tensor_copy(out=ident[:], in_=identf[:]), [identf], [ident])

        rr = [0]

        def evac(out, in_, reads, writes):
            rr[0] += 1
            if rr[0] % 2:
                k.op("act", lambda: nc.scalar.copy(out=out, in_=in_), reads, writes)
            else:
                k.op("dve", lambda: nc.vector.tensor_copy(out=out, in_=in_), reads, writes)

        def cast(e, out, in_, reads, writes):
            if e == "act":
                k.op("act", lambda: nc.scalar.copy(out=out, in_=in_), reads, writes)
            elif e == "pool":
                k.op("pool", lambda: nc.gpsimd.tensor_copy(out=out, in_=in_), reads, writes)
            else:
                k.op("dve", lambda: nc.vector.tensor_copy(out=out, in_=in_), reads, writes)

        def mm(ps, out_ap, lhsT, rhs, start, stop, reads, inc=None):
            if inc is None:
                inc = stop
            k.op("pe", lambda: nc.tensor.matmul(out_ap, lhsT=lhsT, rhs=rhs, start=start, stop=stop),
                 reads, [ps], inc=inc, skip_self=True)

        def mm_acc(ps, out_ap, lhsT, rhs, reads, inc):
            k.op("pe", lambda: nc.tensor.matmul(out_ap, lhsT=lhsT, rhs=rhs, start=False, stop=False,
                                                skip_group_check=True),
                 reads, [ps], inc=inc, skip_self=True)

        def transp(ps, out_ap, in_ap, idt, reads, inc=True):
            k.op("pe", lambda: nc.tensor.transpose(out=out_ap, in_=in_ap, identity=idt[:]),
                 list(reads) + [idt], [ps], inc=inc, skip_self=True)

        def load_w(st_bufs, dst, dst_c0, src_cols, ncols, engs=("pool", "dve")):
            nk = src_cols.shape[0] // 128
            for kc in range(nk):
                sg = st_bufs[kc % len(st_bufs)]
                k.dma("sp", sg[:, 0:ncols], src_cols[kc * 128:(kc + 1) * 128, :], writes=[sg])
                cast(engs[kc % len(engs)], dst[:, kc, dst_c0:dst_c0 + ncols], sg[:, 0:ncols], [sg], [dst])

        def rms_rows(st, xt, ssq, rstd, sqj):
            k.op("act", lambda: nc.scalar.activation(out=sqj[:], in_=xt[:], func=AF.Square, accum_out=ssq[:, 0:1]),
                 [xt], [sqj, ssq])
            k.op("dve", lambda: nc.vector.tensor_scalar(out=rstd[:], in0=ssq[:, 0:1], scalar1=1.0 / D, scalar2=EPS,
                                                        op0=OP.mult, op1=OP.add), [ssq], [rstd])
            k.op("act", lambda: nc.scalar.activation(out=rstd[:], in_=rstd[:], func=AF.Sqrt), [rstd], [rstd])
            k.op("dve", lambda: nc.vector.reciprocal(out=rstd[:], in_=rstd[:]), [rstd], [rstd])

        def norm_transpose(xt, gbc, rstd, hn, hT_buf, hT_view_fn):
            k.op("dve", lambda: nc.vector.scalar_tensor_tensor(out=hn[:], in0=xt[:], scalar=rstd[:, 0:1], in1=gbc[:],
                                                               op0=OP.mult, op1=OP.mult), [xt, rstd, gbc], [hn])
            for half in range(2):
                for j in range(8):
                    kc = half * 8 + j
                    transp(ptr, ptr[:, j * 128:(j + 1) * 128], hn[:, kc * 128:(kc + 1) * 128], ident, [hn],
                           inc=(j == 7))
                evac(hT_view_fn(half * 8, 8), ptr[:].rearrange("p (j t) -> p j t", j=8), [ptr], [hT_buf])

        def p0_gen(stg, cb, engs):
            it = 0
            for src, dst, rows, cols in ((uT_d, Ubf, D, NE), (v_d, Vbf, NE, D)):
                for r0 in range(0, rows, 128):
                    for c0 in range(0, cols, 4096):
                        w = min(4096, cols - c0)
                        s_, c_ = stg[it % 2], cb[it % 2]
                        k.dma("sp", s_[:, 0:w], src[r0:r0 + 128, c0:c0 + w], writes=[s_])
                        cast(engs[it % len(engs)], c_[:, 0:w], s_[:, 0:w], [s_], [c_])
                        k.dma("pool", dst[r0:r0 + 128, c0:c0 + w], c_[:, 0:w], reads=[c_])
                        it += 1
                        yield

        if "0" in phases and "1a" not in phases:
            with ExitStack() as ph:
                stg = [SB(ph, f"p0s{i}", [128, 4096], F32) for i in range(2)]
                cb = [SB(ph, f"p0c{i}", [128, 4096], BF16) for i in range(2)]
                for _ in p0_gen(stg, cb, ("pool", "dve", "act")):
                    pass
            k.barrier()

        if "1a" in phases:
            with ExitStack() as ph:
                wkv = SB(ph, "wkv", [128, KC, 2112], BF16)
                stg = [SB(ph, f"p1s{i}", [128, 1024], F32) for i in range(2)]
                gbc = SB(ph, "gbc", [128, D], F32)
                k.dma("sp", gbc[:], g_mix_d, writes=[gbc])
                load_w(stg, wkv, 0, w_in[:, C_K:C_K + 1024], 1024)
                load_w(stg, wkv, 1024, w_in[:, C_V:C_V + 1024], 1024)
                load_w(stg, wkv, 2048, w_in[:, C_KI:C_KI + 64], 64)
                xin = [SB(ph, f"xin{i}", [128, D], F32) for i in range(2)]
                sqj = SB(ph, "sqj", [128, D], BF16)
                hn = [SB(ph, f"hn{i}", [128, D], BF16) for i in range(2)]
                ssq = [SB(ph, f"ssq{i}", [128, 1], F32) for i in range(2)]
                rstd = [SB(ph, f"rstd{i}", [128, 1], F32) for i in range(2)]
                hT = [SB(ph, f"hT{i}", [128, KC, 512], BF16) for i in range(2)]
                ksb = [SB(ph, f"ksb{i}", [128, 512], BF16) for i in range(2)]
                vsb = [SB(ph, f"vsb{i}", [128, 8, 129], BF16) for i in range(2)]
                for v_ in vsb:
                    k.op("pool", lambda v_=v_: nc.gpsimd.memset(v_[:], 1.0), [], [v_])
                kib = [SB(ph, f"kib{i}", [64, 512], BF16) for i in range(2)]
                n_e = 0
                g0 = None
                if "0" in phases:
                    p0s = [SB(ph, f"p0s{i}", [128, 4096], F32) for i in range(2)]
                    p0c = [SB(ph, f"p0c{i}", [128, 4096], BF16) for i in range(2)]
                    g0 = p0_gen(p0s, p0c, ("pool",))
                    per0 = -(-192 // NST)
                def tick(n):
                    if g0 is not None:
                        for _ in range(n):
                            next(g0, None)

                ticks = [per0 // 6 + (1 if j < per0 % 6 else 0) for j in range(6)] if g0 is not None else [0] * 6
                for st_i in range(NST):
                    hTb = hT[st_i % 2]
                    for ts in range(4):
                        tile = st_i * 4 + ts
                        xt = xin[tile % 2]
                        k.dma("sp", xt[:], x_all[tile * 128:(tile + 1) * 128, :], writes=[xt])
                        rms_rows(ph, xt, ssq[tile % 2], rstd[tile % 2], sqj)
                        norm_transpose(xt, gbc, rstd[tile % 2], hn[tile % 2], hTb,
                                       lambda k0, n, ts=ts, hTb=hTb: hTb[:, k0:k0 + n, ts * 128:(ts + 1) * 128])
                        tick(ticks[ts])
                    for h in range(8):
                        ps = pb[n_e % 4]
                        for kc in range(KC):
                            mm(ps, ps[:, :], wkv[:, kc, h * 128:(h + 1) * 128], hTb[:, kc, :], kc == 0, kc == KC - 1,
                               [wkv, hTb])
                        kb_ = ksb[n_e % 2]
                        evac(kb_[:], ps[:, :], [ps], [kb_])
                        k.dma("pool", KT[h, :, st_i * 512:(st_i + 1) * 512], kb_[:], reads=[kb_])
                        n_e += 1
                    tick(ticks[4])
                    for ts in range(4):
                        vb_ = vsb[ts % 2]
                        for half in range(2):
                            ps = pb[n_e % 4]
                            for kc in range(KC):
                                mm(ps, ps[:, :], hTb[:, kc, ts * 128:(ts + 1) * 128],
                                   wkv[:, kc, 1024 + half * 512:1024 + (half + 1) * 512], kc == 0, kc == KC - 1,
                                   [wkv, hTb])
                            evac(vb_[:, half * 4:(half + 1) * 4, 0:128], ps[:, :].rearrange("p (h d) -> p h d", h=4),
                                 [ps], [vb_])
                            n_e += 1
                        tile = st_i * 4 + ts
                        k.dma("pool", Vs[tile * 128:(tile + 1) * 128, :], vb_[:].rearrange("p h d -> p (h d)"),
                              reads=[vb_])
                    tick(ticks[5])
                    ps = pb[n_e % 4]
                    for kc in range(KC):
                        mm(ps, ps[0:64, :], wkv[:, kc, 2048:2112], hTb[:, kc, :], kc == 0, kc == KC - 1, [wkv, hTb])
                    kb_ = kib[st_i % 2]
                    evac(kb_[:], ps[0:64, :], [ps], [kb_])
                    k.dma("pool", kiT_s[:, st_i * 512:(st_i + 1) * 512], kb_[:], reads=[kb_])
                    n_e += 1
                if g0 is not None:
                    for _ in g0:
                        pass
            k.barrier()

        if "1b" in phases:
            with ExitStack() as ph:
                gbc = SB(ph, "gbc1", [128, D], F32)
                k.dma("sp", gbc[:], g_mix_d, writes=[gbc])
                xin = [SB(ph, f"b_xin{i}", [128, D], F32) for i in range(2)]
                sqj = SB(ph, "b_sqj", [128, D], BF16)
                hn = [SB(ph, f"b_hn{i}", [128, D], BF16) for i in range(2)]
                ssq = [SB(ph, f"b_ssq{i}", [128, 1], F32) for i in range(2)]
                rstd = [SB(ph, f"b_rstd{i}", [128, 1], F32) for i in range(2)]
                hTs = [SB(ph, f"b_hT{i}", [128, KC, 128], BF16) for i in range(2)]
                for i in range(NO):
                    xt = xin[i % 2]
                    hTb = hTs[i % 2]
                    k.dma("sp", xt[:], x_own[i * 128:(i + 1) * 128, :], writes=[xt])
                    rms_rows(ph, xt, ssq[i % 2], rstd[i % 2], sqj)
                    norm_transpose(xt, gbc, rstd[i % 2], hn[i % 2], hTb,
                                   lambda k0, n, hTb=hTb: hTb[:, k0:k0 + n, :])
                    k.dma("pool", hTo[i], hTb[:], reads=[hTb])
                k.barrier()
                stg = [SB(ph, f"b_s{i}", [128, 2048], F32) for i in range(2)]
                wg = SB(ph, "b_wg", [128, KC, 2048], BF16)
                osb = [SB(ph, f"b_o{i}", [128, 2048], BF16) for i in range(2)]
                osf = [SB(ph, f"b_of{i}", [128, 8], F32) for i in range(2)]
                n_e = 0
                load_w(stg, wg, 0, w_in[:, C_Q:C_Q + 1024], 1024)
                load_w(stg, wg, 1024, w_in[:, C_QI:C_QI + 512], 512)
                load_w(stg, wg, 1536, w_in[:, C_WI:C_WI + 8], 8)
                for i in range(NO):
                    hTb = hTs[i % 2]
                    k.dma("sp", hTb[:], hTo[i], writes=[hTb])
                    ob = osb[i % 2]
                    for hq in range(2):
                        ps = pb[n_e % 4]
                        n_e += 1
                        for hh in range(4):
                            h = hq * 4 + hh
                            for kc in range(KC):
                                mm(ps, ps[:, hh * 128:(hh + 1) * 128], wg[:, kc, h * 128:(h + 1) * 128], hTb[:, kc, :],
                                   kc == 0, kc == KC - 1, [wg, hTb], inc=(kc == KC - 1 and hh == 3))
                        evac(ob[:, hq * 512:(hq + 1) * 512], ps[:, :], [ps], [ob])
                    k.dma("pool", qT[i], ob[:, 0:1024].rearrange("p (h t) -> p h t", h=8), reads=[ob])
                    for hq in range(2):
                        ps = pb[n_e % 4]
                        n_e += 1
                        for hh in range(4):
                            h = hq * 4 + hh
                            for kc in range(KC):
                                mm(ps, ps[0:64, hh * 128:(hh + 1) * 128], wg[:, kc, 1024 + h * 64:1024 + (h + 1) * 64],
                                   hTb[:, kc, :], kc == 0, kc == KC - 1, [wg, hTb], inc=(kc == KC - 1 and hh == 3))
                        evac(ob[0:64, 1024 + hq * 512:1024 + (hq + 1) * 512], ps[0:64, :], [ps], [ob])
                    k.dma("pool", qiT[i], ob[0:64, 1024:2048].rearrange("p (h t) -> p h t", h=8), reads=[ob])
                    ps = pb[n_e % 4]
                    n_e += 1
                    for kc in range(KC):
                        mm(ps, ps[:, 0:8], hTb[:, kc, :], wg[:, kc, 1536:1544], kc == 0, kc == KC - 1, [wg, hTb])
                    of = osf[i % 2]
                    evac(of[:], ps[:, 0:8], [ps], [of])
                    k.dma("pool", wi_s[i], of[:], reads=[of])
                for (c0, dsts, fn) in ((C_GU, (gu_s, gv_s), AF.Gelu_apprx_tanh), (C_GA, (sga_s,), AF.Sigmoid),
                                       (C_GB, (sgb_s,), AF.Sigmoid)):
                    load_w(stg, wg, 0, w_in[:, c0:c0 + 2048], 2048)
                    for i in range(NO):
                        hTb = hTs[i % 2]
                        k.dma("sp", hTb[:], hTo[i], writes=[hTb])
                        ob = osb[i % 2]
                        for cc in range(4):
                            ps = pb[n_e % 4]
                            n_e += 1
                            for kc in range(KC):
                                mm(ps, ps[:, :], hTb[:, kc, :], wg[:, kc, cc * 512:(cc + 1) * 512], kc == 0,
                                   kc == KC - 1, [wg, hTb])
                            k.op("act", lambda ps=ps, ob=ob, cc=cc, fn=fn: nc.scalar.activation(
                                out=ob[:, cc * 512:(cc + 1) * 512], in_=ps[:, :], func=fn), [ps], [ob])
                        if len(dsts) == 2:
                            k.dma("pool", dsts[0][i], ob[:, 0:1024], reads=[ob])
                            k.dma("pool", dsts[1][i], ob[:, 1024:2048], reads=[ob])
                        else:
                            k.dma("pool", dsts[0][i], ob[:, :], reads=[ob])
            k.barrier()

        if "2a" in phases:
            with ExitStack() as ph:
                score = SB(ph, "score", [128, S], F32)
                kiT = SB(ph, "kiT", [64, S], BF16)
                k.dma("sp", kiT[:], kiT_s, writes=[kiT])
                cmask = SB(ph, "cmask", [128, 512], F32)
                k.dma("sp", cmask[:], cmask_d, writes=[cmask])
                qTb = [SB(ph, f"qTb{i}", [128, 8, 128], BF16) for i in range(2)]
                qiTb = [SB(ph, f"qiTb{i}", [64, 8, 128], BF16) for i in range(2)]
                wib = [SB(ph, f"wib{i}", [128, 8], F32) for i in range(2)]
                wabs = SB(ph, "wabs", [128, 8], F32)
                wsgn = SB(ph, "wsgn", [128, 8], F32)
                Rb = [SB(ph, f"Rb{i}", [128, 512], F32) for i in range(2)]
                mid = SB(ph, "mid", [128, 1], F32)
                cntb = SB(ph, "cntb", [128, 1], F32)
                cnt2 = SB(ph, "cnt2", [128, 1], F32)
                ajunk = SB(ph, "ajunk", [128, S - max(128, (int(S * 0.45) // 128) * 128)], BF16)
                tmpb = SB(ph, "tmpb", [128, 1], F32)
                mk = [SB(ph, f"mk{i}", [128, 512], BF16) for i in range(2)]
                mkT = [SB(ph, f"mkT{i}", [128, 512], BF16) for i in range(2)]
                Kc = [SB(ph, f"Kc{i}", [128, 8, 512], BF16) for i in range(3)]
                Vc = [SB(ph, f"Vc{i}", [128, 4, 8, 129], BF16) for i in range(3)]
                Eb = [SB(ph, f"Eb{i}", [128, 512], BF16) for i in range(4)]
                Pb = [SB(ph, f"Pb{i}", [128, 512], BF16) for i in range(4)]
                ysb = SB(ph, "ysb", [128, 1024], BF16)
                rz = SB(ph, "rz", [128, 8], F32)
                yTb = [SB(ph, f"yTb{i}", [128, 8, 128], BF16) for i in range(2)]
                ps_idx = [pb[0], pb[1]]
                ps_s = [pb[0], pb[1], pb[2], pb[3]]
                ps_o = [pb[4], pb[5], pb[6]]
                WSC = (8.0 ** -0.5) * (64.0 ** -0.5)
                ASC = 128.0 ** -0.5
                n_i = 0
                n_c = 0
                for i in range(NO):
                    L = (i + 1) * 512
                    nch = L // 512
                    q_, qi_, w_ = qTb[i % 2], qiTb[i % 2], wib[i % 2]
                    k.dma("sp", q_[:], qT[i], writes=[q_])
                    k.dma("sp", qi_[:], qiT[i], writes=[qi_])
                    k.dma("sp", w_[:], wi_s[i], writes=[w_])
                    k.op("act", lambda: nc.scalar.activation(out=wabs[:], in_=w_[:], func=AF.Abs, scale=WSC),
                         [w_], [wabs])
                    k.op("act", lambda: nc.scalar.activation(out=wsgn[:], in_=w_[:], func=AF.Sign), [w_], [wsgn])
                    for c in range(nch):
                        sl = slice(c * 512, (c + 1) * 512)
                        for h in range(8):
                            ps = ps_idx[n_i % 2]
                            R = Rb[n_i % 2]
                            n_i += 1
                            mm(ps, ps[:, :], qi_[:, h, :], kiT[0:64, sl], True, True, [qi_, kiT])
                            k.op("act", lambda ps=ps, R=R, h=h: nc.scalar.activation(
                                out=R[:], in_=ps[:, :], func=AF.Relu, scale=wabs[:, h:h + 1]), [ps, wabs], [R])
                            if h == 0:
                                k.op("dve", lambda R=R, sl=sl: nc.vector.tensor_scalar(
                                    out=score[:, sl], in0=R[:], scalar1=wsgn[:, 0:1], scalar2=None, op0=OP.mult),
                                    [R, wsgn], [score])
                            else:
                                k.op("dve", lambda R=R, sl=sl, h=h: nc.vector.scalar_tensor_tensor(
                                    out=score[:, sl], in0=R[:], scalar=wsgn[:, h:h + 1], in1=score[:, sl],
                                    op0=OP.mult, op1=OP.add), [R, wsgn, score], [score])
                    k.op("dve", lambda: nc.vector.tensor_tensor(out=score[:, L - 512:L], in0=score[:, L - 512:L],
                                                                in1=cmask[:], op=OP.add), [score, cmask], [score])
                    H1 = max(128, (int(L * 0.45) // 128) * 128)
                    n2 = float(L - H1)
                    k.op("dve", lambda: nc.vector.memset(mid[:], 3.14159e-7), [], [mid])
                    for it in range(NBIS):
                        hk = 16.0 * (2.0 ** -it)
                        last = it == NBIS - 1
                        k.op("dve", lambda: nc.vector.tensor_scalar(
                            out=junk[:, 0:1].broadcast_to([128, H1]), in0=score[:, 0:H1], scalar1=mid[:, 0:1],
                            scalar2=0.0, op0=OP.is_ge, op1=OP.add, accum_out=cntb[:, 0:1]), [score, mid],
                            [junk, cntb])
                        k.op("act", lambda: nc.scalar.activation(
                            out=ajunk[:, 0:L - H1], in_=score[:, H1:L], func=AF.Sign, bias=mid[:, 0:1], scale=-1.0,
                            accum_out=cnt2[:, 0:1]), [score, mid], [ajunk, cnt2])
                        k.op("dve", lambda: nc.vector.scalar_tensor_tensor(
                            out=tmpb[:], in0=cntb[:], scalar=2.0, in1=cnt2[:], op0=OP.mult, op1=OP.subtract),
                            [cntb, cnt2], [tmpb])
                        step = (hk / 2.0) if not last else hk
                        mul = 2.0 * step if not last else step
                        k.op("dve", lambda mul=mul: nc.vector.tensor_scalar(
                            out=tmpb[:], in0=tmpb[:], scalar1=511.0 - n2, scalar2=mul, op0=OP.is_ge, op1=OP.mult),
                            [tmpb], [tmpb])
                        k.op("dve", lambda step=step: nc.vector.scalar_tensor_tensor(
                            out=mid[:], in0=tmpb[:], scalar=-step, in1=mid[:], op0=OP.add, op1=OP.add),
                            [tmpb, mid], [mid])
                    for b3 in ps_o:
                        k.op("dve", lambda b3=b3: nc.vector.memset(b3[:, :], 0.0), [], [b3])
                    def load(c):
                        sl = slice(c * 512, (c + 1) * 512)
                        K_, V_ = Kc[c % 3], Vc[c % 3]
                        k.dma("sp", K_[:], KT.rearrange("h p s -> p h s")[:, :, sl], writes=[K_])
                        k.dma("sp", V_[:].rearrange("p b h d -> p b (h d)"),
                              Vs[c * 512:(c + 1) * 512, :].rearrange("(b p) e -> p b e", p=128), writes=[V_])

                    def prep(c):
                        sl = slice(c * 512, (c + 1) * 512)
                        m_, mT_ = mk[c % 2], mkT[c % 2]
                        if c + 1 < nch:
                            load(c + 1)
                        k.op("dve", lambda: nc.vector.tensor_scalar(
                            out=m_[:], in0=score[:, sl], scalar1=mid[:, 0:1], scalar2=None, op0=OP.is_ge),
                            [score, mid], [m_])
                        for b in range(4):
                            transp(ptr, ptr[:, b * 128:(b + 1) * 128], m_[:, b * 128:(b + 1) * 128], ident, [m_])
                        evac(mT_[:], ptr[:, 0:512], [ptr], [mT_])

                    def qk(c, h):
                        ps = ps_s[h % 4]
                        K_ = Kc[c % 3]
                        for b in range(4):
                            mm(ps, ps[:, b * 128:(b + 1) * 128], K_[:, h, b * 128:(b + 1) * 128], q_[:, h, :],
                               True, True, [K_, q_])

                    def rest(c, h):
                        ps = ps_s[h % 4]
                        V_, mT_ = Vc[c % 3], mkT[c % 2]
                        E_, P_ = Eb[h % 4], Pb[h % 4]
                        k.op("act", lambda: nc.scalar.activation(
                            out=E_[:], in_=ps[:, :], func=AF.Exp, scale=ASC), [ps], [E_])
                        k.op("dve", lambda: nc.vector.tensor_tensor(
                            out=P_[:], in0=E_[:], in1=mT_[:], op=OP.mult), [E_, mT_], [P_])
                        po = ps_o[h // 3]
                        oc = (h % 3) * 129
                        for b in range(4):
                            mm_acc(po, po[:, oc:oc + 129], P_[:, b * 128:(b + 1) * 128], V_[:, b, h, :],
                                   [P_, V_], inc=(b == 3))

                    items = [(c, h) for c in range(nch) for h in range(8)]
                    DEPTH = 3
                    prepped = set()

                    def issue_qk(j):
                        if j < len(items):
                            c2, h2 = items[j]
                            if c2 not in prepped:
                                prepped.add(c2)
                                prep(c2)
                            qk(c2, h2)

                    load(0)
                    for j in range(DEPTH):
                        issue_qk(j)
                    for idx, (c, h) in enumerate(items):
                        issue_qk(idx + DEPTH)
                        rest(c, h)
                    for h in range(8):
                        po = ps_o[h // 3]
                        oc = (h % 3) * 129
                        k.op("dve", lambda po=po, oc=oc, h=h: nc.vector.reciprocal(
                            out=rz[:, h:h + 1], in_=po[:, oc + 128:oc + 129]), [po], [rz])
                        k.op("dve", lambda po=po, oc=oc, h=h: nc.vector.tensor_scalar(
                            out=ysb[:, h * 128:(h + 1) * 128], in0=po[:, oc:oc + 128], scalar1=rz[:, h:h + 1],
                            scalar2=None, op0=OP.mult), [po, rz], [ysb])
                    yT_ = yTb[i % 2]
                    for h in range(8):
                        transp(ptr, ptr[:, h * 128:(h + 1) * 128], ysb[:, h * 128:(h + 1) * 128], ident, [ysb],
                               inc=(h == 7))
                    evac(yT_[:], ptr[:].rearrange("p (h t) -> p h t", h=8), [ptr], [yT_])
                    k.dma("pool", yaT[i], yT_[:], reads=[yT_])
            k.barrier()

        if "2b" in phases:
            with ExitStack() as ph:
                stg = [SB(ph, f"c_s{i}", [128, 2048], F32) for i in range(2)]
                wa = SB(ph, "wa", [128, 8, D], BF16)
                wb = SB(ph, "wb", [128, 8, D], BF16)
                load_w(stg, wa, 0, w_ba, 2048)
                load_w(stg, wb, 0, w_bg, 2048)
                wsT = SB(ph, "wsT", [128, 8, 128], BF16)
                trilb = SB(ph, "trilb", [128, 128], F32)
                k.dma("sp", trilb[:], tril_d, writes=[trilb])
                for g in range(8):
                    sg = stg[g % 2]
                    k.dma("sp", sg[:, 0:128], wsT_d[g], writes=[sg])
                    k.op("dve", lambda g=g, sg=sg: nc.vector.tensor_tensor(out=wsT[:, g, :], in0=sg[:, 0:128],
                                                                           in1=trilb[:], op=OP.mult),
                         [sg, trilb], [wsT])
                bT = SB(ph, "bT", [128, 8], F32)
                k.dma("sp", bT[:], bT_d, writes=[bT])
                ggm = SB(ph, "ggm", [128, 1024], F32)
                k.dma("sp", ggm[:], g_gm_d, writes=[ggm])
                gub = [SB(ph, f"gub{i}", [128, 1024], BF16) for i in range(2)]
                gvb = [SB(ph, f"gvb{i}", [128, 1024], BF16) for i in range(2)]
                sab = [SB(ph, f"sab{i}", [128, D], BF16) for i in range(2)]
                sbb = [SB(ph, f"sbb{i}", [128, D], BF16) for i in range(2)]
                yab = [SB(ph, f"yab{i}", [128, 8, 128], BF16) for i in range(2)]
                vsq = SB(ph, "vsq", [128, 1024], BF16)
                vss = SB(ph, "vss", [128, 1], F32)
                vrs = SB(ph, "vrs", [128, 1], F32)
                vn = SB(ph, "vn", [128, 1024], BF16)
                yb = SB(ph, "yb", [128, 1024], BF16)
                ybT = SB(ph, "ybT", [128, 8, 128], BF16)
                mixa = SB(ph, "mixa", [128, D], F32)
                mixc = SB(ph, "mixc", [128, D], F32)
                mixb = SB(ph, "mixb", [128, D], BF16)
                mixTb = [SB(ph, f"mixT{i}", [128, KC, 128], BF16) for i in range(2)]
                n_e = 0
                for i in range(NO):
                    gu_, gv_, sa_, sb_, ya_ = gub[i % 2], gvb[i % 2], sab[i % 2], sbb[i % 2], yab[i % 2]
                    mixT = mixTb[i % 2]
                    k.dma("sp", gu_[:], gu_s[i], writes=[gu_])
                    k.dma("sp", gv_[:], gv_s[i], writes=[gv_])
                    k.dma("sp", sa_[:], sga_s[i], writes=[sa_])
                    k.dma("sp", sb_[:], sgb_s[i], writes=[sb_])
                    k.dma("sp", ya_[:], yaT[i], writes=[ya_])
                    k.op("act", lambda gv_=gv_: nc.scalar.activation(out=vsq[:], in_=gv_[:], func=AF.Square,
                                                                    accum_out=vss[:, 0:1]), [gv_], [vsq, vss])
                    k.op("dve", lambda: nc.vector.tensor_scalar(out=vrs[:], in0=vss[:, 0:1], scalar1=1.0 / 1024,
                                                                scalar2=EPS, op0=OP.mult, op1=OP.add), [vss], [vrs])
                    k.op("act", lambda: nc.scalar.activation(out=vrs[:], in_=vrs[:], func=AF.Sqrt), [vrs], [vrs])
                    k.op("dve", lambda: nc.vector.reciprocal(out=vrs[:], in_=vrs[:]), [vrs], [vrs])
                    k.op("dve", lambda gv_=gv_: nc.vector.scalar_tensor_tensor(
                        out=vn[:], in0=gv_[:], scalar=vrs[:, 0:1], in1=ggm[:], op0=OP.mult, op1=OP.mult),
                        [gv_, vrs, ggm], [vn])
                    pz = [pb[0], pb[1]]
                    for g in range(8):
                        ps = pz[g // 4]
                        mm(ps, ps[:, (g % 4) * 128:(g % 4 + 1) * 128], wsT[:, g, :], vn[:, g * 128:(g + 1) * 128],
                           True, True, [wsT, vn], inc=(g % 4 == 3))
                    for g in range(8):
                        ps = pz[g // 4]
                        k.op("dve", lambda ps=ps, g=g, gu_=gu_: nc.vector.scalar_tensor_tensor(
                            out=yb[:, g * 128:(g + 1) * 128], in0=ps[:, (g % 4) * 128:(g % 4 + 1) * 128],
                            scalar=bT[:, g:g + 1], in1=gu_[:, g * 128:(g + 1) * 128], op0=OP.add, op1=OP.mult),
                            [ps, bT, gu_], [yb])
                    for g in range(8):
                        transp(ptr, ptr[:, g * 128:(g + 1) * 128], yb[:, g * 128:(g + 1) * 128], ident, [yb],
                               inc=(g == 7))
                    evac(ybT[:], ptr[:].rearrange("p (g t) -> p g t", g=8), [ptr], [ybT])
                    for cc in range(4):
                        csl = slice(cc * 512, (cc + 1) * 512)
                        psa = pb[2 + (n_e % 2)]
                        psb = pb[4 + (n_e % 2)]
                        n_e += 1
                        for kc in range(8):
                            mm(psa, psa[:, :], ya_[:, kc, :], wa[:, kc, csl], kc == 0, kc == 7, [ya_, wa])
                        for kc in range(8):
                            mm(psb, psb[:, :], ybT[:, kc, :], wb[:, kc, csl], kc == 0, kc == 7, [ybT, wb])
                        k.op("dve", lambda psa=psa, csl=csl, sa_=sa_: nc.vector.tensor_tensor(
                            out=mixa[:, csl], in0=psa[:, :], in1=sa_[:, csl], op=OP.mult), [psa, sa_], [mixa])
                        k.op("dve", lambda psb=psb, csl=csl, sb_=sb_: nc.vector.tensor_tensor(
                            out=mixc[:, csl], in0=psb[:, :], in1=sb_[:, csl], op=OP.mult), [psb, sb_], [mixc])
                        k.op("pool", lambda csl=csl: nc.gpsimd.tensor_tensor(
                            out=mixb[:, csl], in0=mixc[:, csl], in1=mixa[:, csl], op=OP.add), [mixa, mixc], [mixb])
                    for half in range(2):
                        for j in range(8):
                            kc = half * 8 + j
                            transp(ptr, ptr[:, j * 128:(j + 1) * 128], mixb[:, kc * 128:(kc + 1) * 128], ident,
                                   [mixb], inc=(j == 7))
                        evac(mixT[:, half * 8:half * 8 + 8, :], ptr[:].rearrange("p (j t) -> p j t", j=8), [ptr],
                             [mixT])
                    k.dma("pool", mixT_s[i], mixT[:], reads=[mixT])
            k.barrier()

        if "2c" in phases:
            with ExitStack() as ph:
                stg = [SB(ph, f"c2_s{i}", [128, 2048], F32) for i in range(2)]
                wo = SB(ph, "wo", [128, KC, D], BF16)
                load_w(stg, wo, 0, w_o, 2048)
                xob = [SB(ph, f"xob{i}", [128, D], F32) for i in range(2)]
                mixTb = [SB(ph, f"c2_mT{i}", [128, KC, 128], BF16) for i in range(2)]
                n_e = 0
                for i in range(NO):
                    xo_, mixT = xob[i % 2], mixTb[i % 2]
                    k.dma("sp", xo_[:], x_own[i * 128:(i + 1) * 128, :], writes=[xo_])
                    k.dma("sp", mixT[:], mixT_s[i], writes=[mixT])
                    for cc in range(4):
                        csl = slice(cc * 512, (cc + 1) * 512)
                        ps = pb[n_e % 4]
                        n_e += 1
                        for kc in range(KC):
                            mm(ps, ps[:, :], mixT[:, kc, :], wo[:, kc, csl], kc == 0, kc == KC - 1, [mixT, wo])
                        k.op("dve", lambda ps=ps, csl=csl, xo_=xo_: nc.vector.tensor_tensor(
                            out=xo_[:, csl], in0=ps[:, :], in1=xo_[:, csl], op=OP.add), [ps, xo_], [xo_])
                    k.dma("pool", x1_s[i], xo_[:], reads=[xo_])
            k.barrier()

        if "3a" in phases:
            with ExitStack() as ph:
                stg = [SB(ph, f"d_s{i}", [128, 2048], F32) for i in range(2)]
                wq = SB(ph, "wq", [128, KC, D], BF16)
                load_w(stg, wq, 0, w_pq, 2048)
                skT = SB(ph, "skT", [128, 16, 128], BF16)
                for b in range(16):
                    sg = stg[b % 2]
                    k.dma("sp", sg[:, 0:128], skT_d[b], writes=[sg])
                    cast("dve", skT[:, b, :], sg[:, 0:128], [sg], [skT])
                gbc = SB(ph, "gbc3", [128, D], F32)
                k.dma("sp", gbc[:], g_ffn_d, writes=[gbc])
                xin = [SB(ph, f"d_x{i}", [128, D], F32) for i in range(2)]
                sqj = SB(ph, "d_sqj", [128, D], BF16)
                hn = SB(ph, "d_hn", [128, D], BF16)
                ssq = SB(ph, "d_ssq", [128, 1], F32)
                rstd = SB(ph, "d_rstd", [128, 1], F32)
                hTbs = [SB(ph, f"d_hT{i}", [128, KC, 128], BF16) for i in range(2)]
                qpT = SB(ph, "qpT", [128, 16, 128], BF16)
                ssbs = [SB(ph, f"ssb{i}", [128, 16, 128], F32) for i in range(2)]
                n_e = 0
                for i in range(NO):
                    xt = xin[i % 2]
                    hTb = hTbs[i % 2]
                    ssb = ssbs[i % 2]
                    k.dma("sp", xt[:], x1_s[i], writes=[xt])
                    rms_rows(ph, xt, ssq, rstd, sqj)
                    norm_transpose(xt, gbc, rstd, hn, hTb, lambda k0, n, hTb=hTb: hTb[:, k0:k0 + n, :])
                    k.dma("pool", hn2T[i], hTb[:], reads=[hTb])
                    for bq in range(4):
                        ps = pb[n_e % 2]
                        n_e += 1
                        for bb in range(4):
                            blk = bq * 4 + bb
                            for kc in range(KC):
                                mm(ps, ps[:, bb * 128:(bb + 1) * 128], wq[:, kc, blk * 128:(blk + 1) * 128],
                                   hTb[:, kc, :], kc == 0, kc == KC - 1, [wq, hTb], inc=(kc == KC - 1 and bb == 3))
                        evac(qpT[:, bq * 4:bq * 4 + 4, :], ps[:, :].rearrange("p (b t) -> p b t", b=4), [ps], [qpT])
                    for bq in range(4):
                        ps = pb[2 + (n_e % 2)]
                        n_e += 1
                        for bb in range(4):
                            blk = bq * 4 + bb
                            mm(ps, ps[:, bb * 128:(bb + 1) * 128], qpT[:, blk, :], skT[:, blk, :], True, True,
                               [qpT, skT], inc=(bb == 3))
                        k.op("act", lambda ps=ps, bq=bq, ssb=ssb: nc.scalar.copy(
                            out=ssb[:, bq * 4:bq * 4 + 4, :], in_=ps[:, :].rearrange("p (b n) -> p b n", b=4)),
                            [ps], [ssb])
                    k.dma("pool", s_s[i], ssb[:], reads=[ssb])
            k.barrier()

        if "3b" in phases:
            with ExitStack() as ph:
                iota_i = SB(ph, "iota_i", [128, 128], F32)
                k.dma("sp", iota_i[:], iota_d, writes=[iota_i])
                ssb = SB(ph, "ssb", [128, 16, 128], F32)
                top = SB(ph, "top", [128, 16, 16], F32)
                tix = SB(ph, "tix", [128, 16, 16], U32)
                tixf = SB(ph, "tixf", [128, 16, 16], F32)
                cand = SB(ph, "cand", [128, 8, 256], F32)
                best = SB(ph, "best", [128, 8, 16], F32)
                bix = SB(ph, "bix", [128, 8, 16], U32)
                ba_u = SB(ph, "ba_u", [128, 8, 16], U32)
                bb_u = SB(ph, "bb_u", [128, 8, 16], U32)
                ba_f = SB(ph, "ba_f", [128, 8, 16], F32)
                bb_f = SB(ph, "bb_f", [128, 8, 16], F32)
                eq = SB(ph, "eq", [128, 8, 16, 16], F32)
                isel = SB(ph, "isel", [128, 8, 16], F32)
                jsel = SB(ph, "jsel", [128, 8, 16], F32)
                gate = SB(ph, "gate", [128, 8, 16], F32)
                gsum = SB(ph, "gsum", [128, 8], F32)
                iT = SB(ph, "iT", [128, 128], F32)
                jT = SB(ph, "jT", [128, 128], F32)
                gT = SB(ph, "gT", [128, 128], F32)
                A_all = SB(ph, "A_all", [128, 128, 128], BF16)
                B_all = SB(ph, "B_all", [128, 128, 128], BF16)
                Gt = [SB(ph, f"Gt{i}", [128, 128, 128], BF16) for i in range(1)]
                for i in range(NO):
                    k.dma("sp", ssb[:], s_s[i], writes=[ssb])
                    for blk in range(16):
                        k.op("dve", lambda blk=blk: nc.vector.max(out=top[:, blk, 0:8], in_=ssb[:, blk, :]),
                             [ssb], [top])
                        k.op("dve", lambda blk=blk: nc.vector.max_index(out=tix[:, blk, 0:8], in_max=top[:, blk, 0:8],
                                                                        in_values=ssb[:, blk, :]), [ssb, top], [tix])
                        k.op("dve", lambda blk=blk: nc.vector.match_replace(
                            out=ssb[:, blk, :], in_to_replace=top[:, blk, 0:8], in_values=ssb[:, blk, :],
                            imm_value=NEG), [ssb, top], [ssb])
                        k.op("dve", lambda blk=blk: nc.vector.max(out=top[:, blk, 8:16], in_=ssb[:, blk, :]),
                             [ssb], [top])
                        k.op("dve", lambda blk=blk: nc.vector.max_index(out=tix[:, blk, 8:16],
                                                                        in_max=top[:, blk, 8:16],
                                                                        in_values=ssb[:, blk, :]),
                             [ssb, top], [tix])
                    k.op("dve", lambda: nc.vector.tensor_copy(out=tixf[:], in_=tix[:]), [tix], [tixf])
                    topv = top[:].rearrange("p (h two) k -> p h two k", two=2)
                    k.op("dve", lambda: nc.vector.tensor_tensor(
                        out=cand[:].rearrange("p h (a b) -> p h a b", a=16),
                        in0=topv[:, :, 0, :].unsqueeze(3).broadcast_to([128, 8, 16, 16]),
                        in1=topv[:, :, 1, :].unsqueeze(2).broadcast_to([128, 8, 16, 16]), op=OP.add), [top], [cand])
                    for h in range(8):
                        k.op("dve", lambda h=h: nc.vector.max(out=best[:, h, 0:8], in_=cand[:, h, :]), [cand], [best])
                        k.op("dve", lambda h=h: nc.vector.max_index(out=bix[:, h, 0:8], in_max=best[:, h, 0:8],
                                                                    in_values=cand[:, h, :]), [cand, best], [bix])
                        k.op("dve", lambda h=h: nc.vector.match_replace(
                            out=cand[:, h, :], in_to_replace=best[:, h, 0:8], in_values=cand[:, h, :],
                            imm_value=NEG), [cand, best], [cand])
                        k.op("dve", lambda h=h: nc.vector.max(out=best[:, h, 8:16], in_=cand[:, h, :]),
                             [cand], [best])
                        k.op("dve", lambda h=h: nc.vector.max_index(out=bix[:, h, 8:16], in_max=best[:, h, 8:16],
                                                                    in_values=cand[:, h, :]), [cand, best], [bix])
                    k.op("dve", lambda: nc.vector.tensor_single_scalar(out=ba_u[:], in_=bix[:], scalar=4,
                                                                       op=OP.logical_shift_right), [bix], [ba_u])
                    k.op("dve", lambda: nc.vector.tensor_single_scalar(out=bb_u[:], in_=bix[:], scalar=15,
                                                                       op=OP.bitwise_and), [bix], [bb_u])
                    k.op("dve", lambda: nc.vector.tensor_copy(out=ba_f[:], in_=ba_u[:]), [ba_u], [ba_f])
                    k.op("dve", lambda: nc.vector.tensor_copy(out=bb_f[:], in_=bb_u[:]), [bb_u], [bb_f])
                    tixv = tixf[:].rearrange("p (h two) k -> p h two k", two=2)
                    iota16 = iota_i[:, 0:16].unsqueeze(1).unsqueeze(1).broadcast_to([128, 8, 16, 16])
                    for (sel_f, which, dst) in ((ba_f, 0, isel), (bb_f, 1, jsel)):
                        k.op("dve", lambda sel_f=sel_f: nc.vector.tensor_tensor(
                            out=eq[:], in0=sel_f[:].unsqueeze(3).broadcast_to([128, 8, 16, 16]), in1=iota16,
                            op=OP.is_equal), [sel_f, iota_i], [eq])
                        k.op("dve", lambda which=which: nc.vector.tensor_tensor(
                            out=eq[:], in0=eq[:], in1=tixv[:, :, which, :].unsqueeze(2).broadcast_to([128, 8, 16, 16]),
                            op=OP.mult), [eq, tixf], [eq])
                        k.op("dve", lambda dst=dst: nc.vector.tensor_reduce(out=dst[:], in_=eq[:], axis=AX.X,
                                                                            op=OP.add), [eq], [dst])
                    k.op("dve", lambda: nc.vector.tensor_tensor(
                        out=gate[:], in0=best[:], in1=best[:, :, 0:1].broadcast_to([128, 8, 16]), op=OP.subtract),
                        [best], [gate])
                    k.op("act", lambda: nc.scalar.activation(out=gate[:], in_=gate[:], func=AF.Exp), [gate], [gate])
                    k.op("dve", lambda: nc.vector.tensor_reduce(out=gsum[:], in_=gate[:], axis=AX.X, op=OP.add),
                         [gate], [gsum])
                    k.op("dve", lambda: nc.vector.reciprocal(out=gsum[:], in_=gsum[:]), [gsum], [gsum])
                    k.op("dve", lambda: nc.vector.tensor_tensor(
                        out=gate[:], in0=gate[:], in1=gsum[:].unsqueeze(2).broadcast_to([128, 8, 16]), op=OP.mult),
                        [gate, gsum], [gate])
                    pt_f = pb[4]
                    for (src, dst, col) in ((isel, iT, 0), (jsel, jT, 1), (gate, gT, 2)):
                        k.op("pe", lambda src=src, col=col: nc.tensor.transpose(
                            out=pt_f[:, col * 128:(col + 1) * 128], in_=src[:].rearrange("p h k -> p (h k)"),
                            identity=identf[:]), [src, identf], [pt_f], skip_self=True)
                        evac(dst[:], pt_f[:, col * 128:(col + 1) * 128], [pt_f], [dst])
                    iota_b = iota_i[:].unsqueeze(1).broadcast_to([128, 128, 128])
                    k.op("dve", lambda: nc.vector.tensor_tensor(
                        out=A_all[:], in0=iota_b, in1=iT[:].unsqueeze(2).broadcast_to([128, 128, 128]),
                        op=OP.is_equal), [iota_i, iT], [A_all])
                    k.op("pool", lambda: nc.gpsimd.tensor_tensor(
                        out=A_all[:], in0=A_all[:], in1=gT[:].unsqueeze(2).broadcast_to([128, 128, 128]),
                        op=OP.mult), [A_all, gT], [A_all])
                    k.op("dve", lambda: nc.vector.tensor_tensor(
                        out=B_all[:], in0=iota_b, in1=jT[:].unsqueeze(2).broadcast_to([128, 128, 128]),
                        op=OP.is_equal), [iota_i, jT], [B_all])
                    G_ = Gt[0]
                    for t4 in range(32):
                        ps = pb[5 + (t4 % 2)]
                        for tt in range(4):
                            t = t4 * 4 + tt
                            mm(ps, ps[:, tt * 128:(tt + 1) * 128], B_all[:, t, :], A_all[:, t, :], True, True,
                               [A_all, B_all], inc=(tt == 3))
                        k.op("act", lambda ps=ps, t4=t4: nc.scalar.copy(
                            out=G_[:, :, t4 * 4:t4 * 4 + 4].rearrange("p i t -> p t i"),
                            in_=ps[:, :].rearrange("p (t i) -> p t i", t=4)), [ps], [G_])
                    for q4 in range(4):
                        k.dma("pool", G_s[i][:, q4 * 32:(q4 + 1) * 32, :], G_[:, q4 * 32:(q4 + 1) * 32, :], reads=[G_])
            k.barrier()

        if "4" in phases:
            TT = min(4, NO)
            NTT = NO // TT
            TW = TT * 128
            GB = 4
            with ExitStack() as ph:
                gfin = SB(ph, "gfin", [128, D], F32)
                k.dma("sp", gfin[:], g_fin_d, writes=[gfin])
                hT4 = SB(ph, "hT4", [128, KC, TW], BF16)
                acc = SB(ph, "acc", [128, TT, D], F32)
                Ug = [SB(ph, f"Ug{i}", [128, KC, GB * 128], BF16) for i in range(2)]
                Vg = [SB(ph, f"Vg{i}", [128, GB, D], BF16) for i in range(2)]
                Gg = [SB(ph, f"Gg{i}", [128, GB, TW], BF16) for i in range(2)]
                GA = [SB(ph, f"GA{i}", [128, GB, TW], BF16) for i in range(2)]
                gl = [SB(ph, f"gl{i}", [128, TW], BF16) for i in range(2)]
                sqj = SB(ph, "e_sqj", [128, D], BF16)
                ssq = SB(ph, "e_ssq", [128, 1], F32)
                rstd = SB(ph, "e_rstd", [128, 1], F32)
                ob = [SB(ph, f"e_ob{i}", [128, D], F32) for i in range(2)]
                n_g = 0
                n_e = 0
                for tt_i in range(NTT):
                    for s_ in range(TT):
                        i = tt_i * TT + s_
                        k.dma("sp", hT4[:, :, s_ * 128:(s_ + 1) * 128], hn2T[i], writes=[hT4])
                        k.dma("sp", acc[:, s_, :], x1_s[i], writes=[acc])
                    for g in range(128 // GB):
                        U_, V_, G_, A_ = Ug[n_g % 2], Vg[n_g % 2], Gg[n_g % 2], GA[n_g % 2]
                        n_g += 1
                        e0 = g * GB * 128
                        k.dma("sp", U_[:], Ubf[:, e0:e0 + GB * 128].rearrange("(kc p) e -> p kc e", p=128),
                              writes=[U_])
                        k.dma("sp", V_[:], Vbf[e0:e0 + GB * 128, :].rearrange("(b p) d -> p b d", p=128),
                              writes=[V_])
                        for s_ in range(TT):
                            i = tt_i * TT + s_
                            k.dma("sp", G_[:, :, s_ * 128:(s_ + 1) * 128], G_s[i][:, g * GB:(g + 1) * GB, :],
                                  writes=[G_])
                        for b in range(GB):
                            ps = pb[n_e % 2]
                            n_e += 1
                            for kc in range(KC):
                                mm(ps, ps[:, 0:TW], U_[:, kc, b * 128:(b + 1) * 128], hT4[:, kc, :], kc == 0,
                                   kc == KC - 1, [U_, hT4])
                            g_ = gl[b % 2]
                            k.op("act", lambda ps=ps, g_=g_: nc.scalar.activation(
                                out=g_[:], in_=ps[:, 0:TW], func=AF.Gelu_apprx_tanh), [ps], [g_])
                            k.op("dve", lambda g_=g_, b=b, A_=A_, G_=G_: nc.vector.tensor_tensor(
                                out=A_[:, b, :], in0=g_[:], in1=G_[:, b, :], op=OP.mult), [g_, G_], [A_])
                        for s_ in range(TT):
                            for cc in range(4):
                                csl = slice(cc * 512, (cc + 1) * 512)
                                ps = pb[2 + (n_e % 4)]
                                n_e += 1
                                for b in range(GB):
                                    mm(ps, ps[:, :], A_[:, b, s_ * 128:(s_ + 1) * 128], V_[:, b, csl], b == 0,
                                       b == GB - 1, [A_, V_])
                                k.op("dve", lambda ps=ps, s_=s_, csl=csl: nc.vector.tensor_tensor(
                                    out=acc[:, s_, csl], in0=ps[:, :], in1=acc[:, s_, csl], op=OP.add),
                                    [ps, acc], [acc])
                    for s_ in range(TT):
                        i = tt_i * TT + s_
                        o_ = ob[s_ % 2]
                        k.op("act", lambda s_=s_: nc.scalar.activation(out=sqj[:], in_=acc[:, s_, :], func=AF.Square,
                                                                       accum_out=ssq[:, 0:1]), [acc], [sqj, ssq])
                        k.op("dve", lambda: nc.vector.tensor_scalar(out=rstd[:], in0=ssq[:, 0:1], scalar1=1.0 / D,
                                                                    scalar2=EPS, op0=OP.mult, op1=OP.add),
                             [ssq], [rstd])
                        k.op("act", lambda: nc.scalar.activation(out=rstd[:], in_=rstd[:], func=AF.Sqrt),
                             [rstd], [rstd])
                        k.op("dve", lambda: nc.vector.reciprocal(out=rstd[:], in_=rstd[:]), [rstd], [rstd])
                        k.op("dve", lambda s_=s_, o_=o_: nc.vector.scalar_tensor_tensor(
                            out=o_[:], in0=acc[:, s_, :], scalar=rstd[:, 0:1], in1=gfin[:], op0=OP.mult, op1=OP.mult),
                            [acc, rstd, gfin], [o_])
                        k.dma("pool", out_d[i * 128:(i + 1) * 128, :], o_[:], reads=[o_])
            k.barrier()
        else:
            pass
        k.barrier()
        nc._kb_inst = k.n_inst
        nc._kb_want = k.want
    return nc


def build2(S, phases=None, expose=()):
    dry = build(S, phases, expose, None)
    return build(S, phases, expose, dry._kb_want)


def _host_inputs(inp, S, n_cores=8, phases=None):
    x = np.asarray(inp["x"], np.float32)
    B = x.shape[0]
    NT = S // 128
    NO = NT // 4
    f = lambda a: np.ascontiguousarray(np.asarray(a, np.float32))
    bc = lambda v, n: f(np.broadcast_to(np.asarray(v, np.float32).reshape(1, n), (128, n)))
    common = {
        "tril": f(np.triu(np.ones((128, 128), np.float32))),
        "ident": f(np.eye(128, dtype=np.float32)),
        "iota": f(np.broadcast_to(np.arange(128, dtype=np.float32)[None, :], (128, 128))),
        "g_mix": bc(inp["ln_mix_g"][0], D),
        "g_ffn": bc(inp["ln_ffn_g"][0], D),
        "g_fin": bc(inp["ln_final_g"], D),
        "g_gm": bc(inp["gmlp_norm_g"][0], 1024),
        "w_in": f(inp["w_in"][0]),
        "wsT": f(np.transpose(inp["w_spatial"][0], (0, 2, 1))),
        "bT": f(np.transpose(inp["b_spatial"][0], (1, 0))),
        "w_ba": f(inp["w_branch_attn"][0]),
        "w_bg": f(inp["w_branch_gmlp"][0]),
        "w_o": f(inp["w_out"][0]),
        "w_pq": f(inp["peer_w_q"][0]),
        "skT": f(np.transpose(np.asarray(inp["peer_sub_keys"][0]).reshape(16, 128, 128), (0, 2, 1))),
    }
    if phases is None or "0" in phases:
        common["uT"] = f(np.asarray(inp["peer_u"][0]).T)
        common["pv"] = f(inp["peer_v"][0])
    maps = []
    for c in range(n_cores):
        b, r = c // 4, c % 4
        xb = x[b % B, :S]
        own = np.concatenate([xb[(4 * i + r) * 128:(4 * i + r + 1) * 128] for i in range(NO)], axis=0)
        sp = np.arange(512)[None, :]
        tq = np.arange(128)[:, None]
        cm = np.where(sp <= r * 128 + tq, 0.0, NEG).astype(np.float32)
        m = dict(common)
        m["x_all"] = f(xb)
        m["x_own"] = f(own)
        m["cmask"] = f(cm)
        maps.append(m)
    return maps


def _gather(results, S, B=2):
    NT = S // 128
    NO = NT // 4
    out = np.zeros((B, S, D), np.float32)
    for c, rmap in enumerate(results):
        b, r = c // 4, c % 4
        o = np.asarray(rmap["out"])
        for i in range(NO):
            out[b, (4 * i + r) * 128:(4 * i + r + 1) * 128] = o[i * 128:(i + 1) * 128]
    return out


def kernel(**inputs):
    S = inputs["x"].shape[1]
    nc = build2(S)
    maps = _host_inputs(inputs, S)
    res = run_bass_kernel_spmd(nc, maps, core_ids=list(range(8)))
    return _gather(res.results, S)
```

```python
from contextlib import ExitStack
import numpy as np
import concourse.bass as bass
import concourse.mybir as mybir
from concourse.bass_utils import run_bass_kernel_spmd

F32 = mybir.dt.float32
BF16 = mybir.dt.bfloat16
U32 = mybir.dt.uint32
AF = mybir.ActivationFunctionType
OP = mybir.AluOpType
AX = mybir.AxisListType

D = 2048
KC = 16
NE = 16384
EPS = 1e-6
NEG = -1.0e30
C_Q, C_K, C_V, C_QI, C_KI, C_WI, C_GU, C_GV, C_GA, C_GB = 0, 1024, 2048, 3072, 3584, 3648, 3656, 4680, 5704, 7752
NBIS = 21


class Dep:
    __slots__ = ("w", "r")

    def __init__(self):
        self.w = None
        self.r = []


class Buf:
    def __init__(self, t):
        self.t = t
        self.d = Dep()

    def __getitem__(self, idx):
        return self.t[idx]


class KB:
    def __init__(self, nc, stack, needed=None, n_dma_sems=20):
        self.nc = nc
        self.dry = needed is None
        self.engs = {"pe": nc.tensor, "act": nc.scalar, "dve": nc.vector,
                     "pool": nc.gpsimd, "sp": nc.sync}
        self.sem = {}
        self.cnt = {}
        for e in self.engs:
            self.sem[e] = stack.enter_context(nc.semaphore("s_" + e))
            self.cnt[e] = 0
        self.dsem = {}
        self.dnext = {}
        for q in ("sp", "pool", "act"):
            lst = []
            for i in range(n_dma_sems):
                key = f"d_{q}_{i}"
                self.sem[key] = stack.enter_context(nc.semaphore(key))
                self.cnt[key] = 0
                lst.append(key)
            self.dsem[q] = lst
            self.dnext[q] = 0
        self.seen = {e: {} for e in self.engs}
        self.snap = {}
        self.want = {e: set() for e in self.engs}
        if needed is not None:
            self.rank = {e: {n: i + 1 for i, n in enumerate(sorted(needed[e]))} for e in self.engs}
        self.n_inst = 0

    def _wait(self, e, key, val):
        if val <= 0 or self.seen[e].get(key, 0) >= val:
            return
        se = self.seen[e]
        se[key] = val
        sn = self.snap.get((key, val))
        if sn:
            for kk, vv in sn.items():
                if se.get(kk, 0) < vv:
                    se[kk] = vv
        self.n_inst += 1
        if key in self.engs:
            if self.dry:
                self.want[key].add(val)
            else:
                self.engs[e].wait_ge(self.sem[key], self.rank[key][val])
        elif not self.dry:
            self.engs[e].wait_ge(self.sem[key], val)

    def _deps(self, e, reads, writes, skip_self):
        need = {}

        def add(tok):
            if skip_self and tok[0] == e:
                return
            if need.get(tok[0], 0) < tok[1]:
                need[tok[0]] = tok[1]

        for b in reads:
            if b.d.w is not None:
                add(b.d.w)
        for b in writes:
            d = b.d
            if d.w is not None:
                add(d.w)
            for rr in d.r:
                add(rr)
        for key, val in need.items():
            self._wait(e, key, val)

    def _mark(self, tok, reads, writes):
        for b in reads:
            d = b.d
            d.r.append(tok)
            if len(d.r) > 48:
                mx = {}
                for kk, v in d.r:
                    if mx.get(kk, 0) < v:
                        mx[kk] = v
                d.r = list(mx.items())
        for b in writes:
            b.d.w = tok
            b.d.r = []

    def op(self, e, fn, reads=(), writes=(), inc=True, skip_self=False):
        self._deps(e, reads, writes, skip_self)
        self.cnt[e] += 1
        n = self.cnt[e]
        self.n_inst += 1
        if not self.dry:
            ins = fn()
            if n in self.rank[e]:
                ins.then_inc(self.sem[e], 1)
        tok = (e, n)
        sn = dict(self.seen[e])
        sn[e] = n - 1
        self.snap[tok] = sn
        self._mark(tok, reads, writes)

    def dma(self, q, out, in_, reads=(), writes=()):
        lst = self.dsem[q]
        key = lst[self.dnext[q] % len(lst)]
        self.dnext[q] += 1
        self._wait(q, key, self.cnt[key])
        self._deps(q, reads, writes, False)
        self.cnt[key] += 16
        if not self.dry:
            ins = self.engs[q].dma_start(out=out, in_=in_)
            ins.then_inc(self.sem[key], 16)
        tok = (key, self.cnt[key])
        self.snap[tok] = dict(self.seen[q])
        self._mark(tok, reads, writes)
        self.n_inst += 1

    def barrier(self):
        for e in self.engs:
            for key in self.sem:
                if key != e:
                    self._wait(e, key, self.cnt[key])
        self.snap = {}


def build(S, phases=None, expose=(), needed=None):
    if phases is None:
        phases = {"0", "1a", "1b", "2a", "2b", "2c", "3a", "3b", "4"}
    NT = S // 128
    NO = NT // 4
    NST = S // 512
    nc = bass.Bass("TRN2", target_bir_lowering=False)

    def din(name, shape, dt=F32):
        return nc.dram_tensor(name, shape, dt, kind="ExternalInput").ap()

    def dsc(name, shape, dt):
        kind = "ExternalOutput" if name in expose else "Internal"
        return nc.dram_tensor(name, shape, dt, kind=kind).ap()

    x_all = din("x_all", [S, D])
    x_own = din("x_own", [NO * 128, D])
    cmask_d = din("cmask", [128, 512])
    tril_d = din("tril", [128, 128])
    ident_d = din("ident", [128, 128])
    iota_d = din("iota", [128, 128])
    g_mix_d = din("g_mix", [128, D])
    g_ffn_d = din("g_ffn", [128, D])
    g_fin_d = din("g_fin", [128, D])
    g_gm_d = din("g_gm", [128, 1024])
    w_in = din("w_in", [D, 9800])
    wsT_d = din("wsT", [8, 128, 128])
    bT_d = din("bT", [128, 8])
    w_ba = din("w_ba", [1024, D])
    w_bg = din("w_bg", [1024, D])
    w_o = din("w_o", [D, D])
    w_pq = din("w_pq", [D, D])
    skT_d = din("skT", [16, 128, 128])
    if "0" in phases:
        uT_d = din("uT", [D, NE])
        v_d = din("pv", [NE, D])
    out_d = nc.dram_tensor("out", [NO * 128, D], F32, kind="ExternalOutput").ap()

    Ubf = dsc("Ubf", [D, NE], BF16)
    Vbf = dsc("Vbf", [NE, D], BF16)
    KT = dsc("KT", [8, 128, S], BF16)
    Vs = dsc("Vs", [S, 8 * 129], BF16)
    hTo = dsc("hTo", [NO, 128, KC, 128], BF16)
    qT = dsc("qT", [NO, 128, 8, 128], BF16)
    qiT = dsc("qiT", [NO, 64, 8, 128], BF16)
    wi_s = dsc("wi_s", [NO, 128, 8], F32)
    gu_s = dsc("gu_s", [NO, 128, 1024], BF16)
    gv_s = dsc("gv_s", [NO, 128, 1024], BF16)
    sga_s = dsc("sga_s", [NO, 128, D], BF16)
    sgb_s = dsc("sgb_s", [NO, 128, D], BF16)
    yaT = dsc("yaT", [NO, 128, 8, 128], BF16)
    x1_s = dsc("x1_s", [NO, 128, D], F32)
    hn2T = dsc("hn2T", [NO, 128, KC, 128], BF16)
    G_s = dsc("G_s", [NO, 128, 128, 128], BF16)
    kiT_s = dsc("kiT_s", [64, S], BF16)
    mixT_s = dsc("mixT_s", [NO, 128, KC, 128], BF16)
    s_s = dsc("s_s", [NO, 128, 16, 128], F32)

    with ExitStack() as top:
        k = KB(nc, top, needed)

        def SB(st, name, shape, dt):
            return Buf(st.enter_context(nc.sbuf_tensor("sb_" + name, shape, dt)))

        def PS(st, name, shape, dt):
            return Buf(st.enter_context(nc.psum_tensor("ps_" + name, shape, dt)))

        pb = [PS(top, f"pb{i}", [128, 512], F32) for i in range(7)]
        ptr = PS(top, "ptr", [128, 1024], BF16)

        ident = SB(top, "ident", [128, 128], BF16)
        identf = SB(top, "identf", [128, 128], F32)
        junk = SB(top, "junk", [128, 8], BF16)
        k.dma("sp", identf[:], ident_d, writes=[identf])
        k.op("dve", lambda: nc.vector.tensor_copy(out=ident[:], in_=identf[:]), [identf], [ident])

        rr = [0]

        def evac(out, in_, reads, writes):
            rr[0] += 1
            if rr[0] % 2:
                k.op("act", lambda: nc.scalar.copy(out=out, in_=in_), reads, writes)
            else:
                k.op("dve", lambda: nc.vector.tensor_copy(out=out, in_=in_), reads, writes)

        def cast(e, out, in_, reads, writes):
            if e == "act":
                k.op("act", lambda: nc.scalar.copy(out=out, in_=in_), reads, writes)
            elif e == "pool":
                k.op("pool", lambda: nc.gpsimd.tensor_copy(out=out, in_=in_), reads, writes)
            else:
                k.op("dve", lambda: nc.vector.tensor_copy(out=out, in_=in_), reads, writes)

        def mm(ps, out_ap, lhsT, rhs, start, stop, reads, inc=None):
            if inc is None:
                inc = stop
            k.op("pe", lambda: nc.tensor.matmul(out_ap, lhsT=lhsT, rhs=rhs, start=start, stop=stop),
                 reads, [ps], inc=inc, skip_self=True)

        def mm_acc(ps, out_ap, lhsT, rhs, reads, inc):
            k.op("pe", lambda: nc.tensor.matmul(out_ap, lhsT=lhsT, rhs=rhs, start=False, stop=False,
                                                skip_group_check=True),
                 reads, [ps], inc=inc, skip_self=True)

        def transp(ps, out_ap, in_ap, idt, reads, inc=True):
            k.op("pe", lambda: nc.tensor.transpose(out=out_ap, in_=in_ap, identity=idt[:]),
                 list(reads) + [idt], [ps], inc=inc, skip_self=True)

        def load_w(st_bufs, dst, dst_c0, src_cols, ncols, engs=("act", "dve")):
            nk = src_cols.shape[0] // 128
            for kc in range(nk):
                sg = st_bufs[kc % len(st_bufs)]
                k.dma("sp", sg[:, 0:ncols], src_cols[kc * 128:(kc + 1) * 128, :], writes=[sg])
                cast(engs[kc % len(engs)], dst[:, kc, dst_c0:dst_c0 + ncols], sg[:, 0:ncols], [sg], [dst])

        def rms_rows(st, xt, ssq, rstd, sqj):
            k.op("act", lambda: nc.scalar.activation(out=sqj[:], in_=xt[:], func=AF.Square, accum_out=ssq[:, 0:1]),
                 [xt], [sqj, ssq])
            k.op("dve", lambda: nc.vector.tensor_scalar(out=rstd[:], in0=ssq[:, 0:1], scalar1=1.0 / D, scalar2=EPS,
                                                        op0=OP.mult, op1=OP.add), [ssq], [rstd])
            k.op("act", lambda: nc.scalar.activation(out=rstd[:], in_=rstd[:], func=AF.Sqrt), [rstd], [rstd])
            k.op("dve", lambda: nc.vector.reciprocal(out=rstd[:], in_=rstd[:]), [rstd], [rstd])

        def norm_transpose(xt, gbc, rstd, hn, hT_buf, hT_view_fn):
            k.op("dve", lambda: nc.vector.scalar_tensor_tensor(out=hn[:], in0=xt[:], scalar=rstd[:, 0:1], in1=gbc[:],
                                                               op0=OP.mult, op1=OP.mult), [xt, rstd, gbc], [hn])
            for half in range(2):
                for j in range(8):
                    kc = half * 8 + j
                    transp(ptr, ptr[:, j * 128:(j + 1) * 128], hn[:, kc * 128:(kc + 1) * 128], ident, [hn],
                           inc=(j == 7))
                evac(hT_view_fn(half * 8, 8), ptr[:].rearrange("p (j t) -> p j t", j=8), [ptr], [hT_buf])

        def p0_gen(stg, cb, engs):
            it = 0
            for src, dst, rows, cols in ((uT_d, Ubf, D, NE), (v_d, Vbf, NE, D)):
                for r0 in range(0, rows, 128):
                    for c0 in range(0, cols, 4096):
                        w = min(4096, cols - c0)
                        s_, c_ = stg[it % 2], cb[it % 2]
                        k.dma("sp", s_[:, 0:w], src[r0:r0 + 128, c0:c0 + w], writes=[s_])
                        cast(engs[it % len(engs)], c_[:, 0:w], s_[:, 0:w], [s_], [c_])
                        k.dma("pool", dst[r0:r0 + 128, c0:c0 + w], c_[:, 0:w], reads=[c_])
                        it += 1
                        yield

        if "0" in phases and "1a" not in phases:
            with ExitStack() as ph:
                stg = [SB(ph, f"p0s{i}", [128, 4096], F32) for i in range(2)]
                cb = [SB(ph, f"p0c{i}", [128, 4096], BF16) for i in range(2)]
                for _ in p0_gen(stg, cb, ("pool", "dve", "act")):
                    pass
            k.barrier()

        if "1a" in phases:
            with ExitStack() as ph:
                wkv = SB(ph, "wkv", [128, KC, 2112], BF16)
                stg = [SB(ph, f"p1s{i}", [128, 1024], F32) for i in range(2)]
                gbc = SB(ph, "gbc", [128, D], F32)
                k.dma("sp", gbc[:], g_mix_d, writes=[gbc])
                load_w(stg, wkv, 0, w_in[:, C_K:C_K + 1024], 1024)
                load_w(stg, wkv, 1024, w_in[:, C_V:C_V + 1024], 1024)
                load_w(stg, wkv, 2048, w_in[:, C_KI:C_KI + 64], 64)
                xin = [SB(ph, f"xin{i}", [128, D], F32) for i in range(2)]
                sqj = SB(ph, "sqj", [128, D], BF16)
                hn = [SB(ph, f"hn{i}", [128, D], BF16) for i in range(2)]
                ssq = [SB(ph, f"ssq{i}", [128, 1], F32) for i in range(2)]
                rstd = [SB(ph, f"rstd{i}", [128, 1], F32) for i in range(2)]
                hT = [SB(ph, f"hT{i}", [128, KC, 512], BF16) for i in range(2)]
                ksb = [SB(ph, f"ksb{i}", [128, 512], BF16) for i in range(2)]
                vsb = [SB(ph, f"vsb{i}", [128, 8, 129], BF16) for i in range(2)]
                for v_ in vsb:
                    k.op("pool", lambda v_=v_: nc.gpsimd.memset(v_[:], 1.0), [], [v_])
                kib = [SB(ph, f"kib{i}", [64, 512], BF16) for i in range(2)]
                n_e = 0
                g0 = None
                if "0" in phases:
                    p0s = [SB(ph, f"p0s{i}", [128, 4096], F32) for i in range(2)]
                    p0c = [SB(ph, f"p0c{i}", [128, 4096], BF16) for i in range(2)]
                    g0 = p0_gen(p0s, p0c, ("act", "pool", "act"))
                    per0 = -(-192 // NST)
                def tick(n):
                    if g0 is not None:
                        for _ in range(n):
                            next(g0, None)

                ticks = [per0 // 6 + (1 if j < per0 % 6 else 0) for j in range(6)] if g0 is not None else [0] * 6
                for st_i in range(NST):
                    hTb = hT[st_i % 2]
                    for ts in range(4):
                        tile = st_i * 4 + ts
                        xt = xin[tile % 2]
                        k.dma("sp", xt[:], x_all[tile * 128:(tile + 1) * 128, :], writes=[xt])
                        rms_rows(ph, xt, ssq[tile % 2], rstd[tile % 2], sqj)
                        norm_transpose(xt, gbc, rstd[tile % 2], hn[tile % 2], hTb,
                                       lambda k0, n, ts=ts, hTb=hTb: hTb[:, k0:k0 + n, ts * 128:(ts + 1) * 128])
                        tick(ticks[ts])
                    for h in range(8):
                        ps = pb[n_e % 4]
                        for kc in range(KC):
                            mm(ps, ps[:, :], wkv[:, kc, h * 128:(h + 1) * 128], hTb[:, kc, :], kc == 0, kc == KC - 1,
                               [wkv, hTb])
                        kb_ = ksb[n_e % 2]
                        evac(kb_[:], ps[:, :], [ps], [kb_])
                        k.dma("pool", KT[h, :, st_i * 512:(st_i + 1) * 512], kb_[:], reads=[kb_])
                        n_e += 1
                    tick(ticks[4])
                    for ts in range(4):
                        vb_ = vsb[ts % 2]
                        for half in range(2):
                            ps = pb[n_e % 4]
                            for kc in range(KC):
                                mm(ps, ps[:, :], hTb[:, kc, ts * 128:(ts + 1) * 128],
                                   wkv[:, kc, 1024 + half * 512:1024 + (half + 1) * 512], kc == 0, kc == KC - 1,
                                   [wkv, hTb])
                            evac(vb_[:, half * 4:(half + 1) * 4, 0:128], ps[:, :].rearrange("p (h d) -> p h d", h=4),
                                 [ps], [vb_])
                            n_e += 1
                        tile = st_i * 4 + ts
                        k.dma("pool", Vs[tile * 128:(tile + 1) * 128, :], vb_[:].rearrange("p h d -> p (h d)"),
                              reads=[vb_])
                    tick(ticks[5])
                    ps = pb[n_e % 4]
                    for kc in range(KC):
                        mm(ps, ps[0:64, :], wkv[:, kc, 2048:2112], hTb[:, kc, :], kc == 0, kc == KC - 1, [wkv, hTb])
                    kb_ = kib[st_i % 2]
                    evac(kb_[:], ps[0:64, :], [ps], [kb_])
                    k.dma("pool", kiT_s[:, st_i * 512:(st_i + 1) * 512], kb_[:], reads=[kb_])
                    n_e += 1
                if g0 is not None:
                    for _ in g0:
                        pass
            k.barrier()

        if "1b" in phases:
            with ExitStack() as ph:
                gbc = SB(ph, "gbc1", [128, D], F32)
                k.dma("sp", gbc[:], g_mix_d, writes=[gbc])
                xin = [SB(ph, f"b_xin{i}", [128, D], F32) for i in range(2)]
                sqj = SB(ph, "b_sqj", [128, D], BF16)
                hn = [SB(ph, f"b_hn{i}", [128, D], BF16) for i in range(2)]
                ssq = [SB(ph, f"b_ssq{i}", [128, 1], F32) for i in range(2)]
                rstd = [SB(ph, f"b_rstd{i}", [128, 1], F32) for i in range(2)]
                hTs = [SB(ph, f"b_hT{i}", [128, KC, 128], BF16) for i in range(2)]
                for i in range(NO):
                    xt = xin[i % 2]
                    hTb = hTs[i % 2]
                    k.dma("sp", xt[:], x_own[i * 128:(i + 1) * 128, :], writes=[xt])
                    rms_rows(ph, xt, ssq[i % 2], rstd[i % 2], sqj)
                    norm_transpose(xt, gbc, rstd[i % 2], hn[i % 2], hTb,
                                   lambda k0, n, hTb=hTb: hTb[:, k0:k0 + n, :])
                    k.dma("pool", hTo[i], hTb[:], reads=[hTb])
                k.barrier()
                stg = [SB(ph, f"b_s{i}", [128, 2048], F32) for i in range(2)]
                wg = SB(ph, "b_wg", [128, KC, 2048], BF16)
                osb = [SB(ph, f"b_o{i}", [128, 2048], BF16) for i in range(2)]
                osf = [SB(ph, f"b_of{i}", [128, 8], F32) for i in range(2)]
                n_e = 0
                load_w(stg, wg, 0, w_in[:, C_Q:C_Q + 1024], 1024)
                load_w(stg, wg, 1024, w_in[:, C_QI:C_QI + 512], 512)
                load_w(stg, wg, 1536, w_in[:, C_WI:C_WI + 8], 8)
                for i in range(NO):
                    hTb = hTs[i % 2]
                    k.dma("sp", hTb[:], hTo[i], writes=[hTb])
                    ob = osb[i % 2]
                    for hq in range(2):
                        ps = pb[n_e % 4]
                        n_e += 1
                        for hh in range(4):
                            h = hq * 4 + hh
                            for kc in range(KC):
                                mm(ps, ps[:, hh * 128:(hh + 1) * 128], wg[:, kc, h * 128:(h + 1) * 128], hTb[:, kc, :],
                                   kc == 0, kc == KC - 1, [wg, hTb], inc=(kc == KC - 1 and hh == 3))
                        evac(ob[:, hq * 512:(hq + 1) * 512], ps[:, :], [ps], [ob])
                    k.dma("pool", qT[i], ob[:, 0:1024].rearrange("p (h t) -> p h t", h=8), reads=[ob])
                    for hq in range(2):
                        ps = pb[n_e % 4]
                        n_e += 1
                        for hh in range(4):
                            h = hq * 4 + hh
                            for kc in range(KC):
                                mm(ps, ps[0:64, hh * 128:(hh + 1) * 128], wg[:, kc, 1024 + h * 64:1024 + (h + 1) * 64],
                                   hTb[:, kc, :], kc == 0, kc == KC - 1, [wg, hTb], inc=(kc == KC - 1 and hh == 3))
                        evac(ob[0:64, 1024 + hq * 512:1024 + (hq + 1) * 512], ps[0:64, :], [ps], [ob])
                    k.dma("pool", qiT[i], ob[0:64, 1024:2048].rearrange("p (h t) -> p h t", h=8), reads=[ob])
                    ps = pb[n_e % 4]
                    n_e += 1
                    for kc in range(KC):
                        mm(ps, ps[:, 0:8], hTb[:, kc, :], wg[:, kc, 1536:1544], kc == 0, kc == KC - 1, [wg, hTb])
                    of = osf[i % 2]
                    evac(of[:], ps[:, 0:8], [ps], [of])
                    k.dma("pool", wi_s[i], of[:], reads=[of])
                for (c0, dsts, fn) in ((C_GU, (gu_s, gv_s), AF.Gelu_apprx_tanh), (C_GA, (sga_s,), AF.Sigmoid),
                                       (C_GB, (sgb_s,), AF.Sigmoid)):
                    load_w(stg, wg, 0, w_in[:, c0:c0 + 2048], 2048)
                    for i in range(NO):
                        hTb = hTs[i % 2]
                        k.dma("sp", hTb[:], hTo[i], writes=[hTb])
                        ob = osb[i % 2]
                        for cc in range(4):
                            ps = pb[n_e % 4]
                            n_e += 1
                            for kc in range(KC):
                                mm(ps, ps[:, :], hTb[:, kc, :], wg[:, kc, cc * 512:(cc + 1) * 512], kc == 0,
                                   kc == KC - 1, [wg, hTb])
                            k.op("act", lambda ps=ps, ob=ob, cc=cc, fn=fn: nc.scalar.activation(
                                out=ob[:, cc * 512:(cc + 1) * 512], in_=ps[:, :], func=fn), [ps], [ob])
                        if len(dsts) == 2:
                            k.dma("pool", dsts[0][i], ob[:, 0:1024], reads=[ob])
                            k.dma("pool", dsts[1][i], ob[:, 1024:2048], reads=[ob])
                        else:
                            k.dma("pool", dsts[0][i], ob[:, :], reads=[ob])
            k.barrier()

        if "2a" in phases:
            with ExitStack() as ph:
                score = SB(ph, "score", [128, S], F32)
                kiT = SB(ph, "kiT", [64, S], BF16)
                k.dma("sp", kiT[:], kiT_s, writes=[kiT])
                cmask = SB(ph, "cmask", [128, 512], F32)
                k.dma("sp", cmask[:], cmask_d, writes=[cmask])
                qTb = [SB(ph, f"qTb{i}", [128, 8, 128], BF16) for i in range(2)]
                qiTb = [SB(ph, f"qiTb{i}", [64, 8, 128], BF16) for i in range(2)]
                wib = [SB(ph, f"wib{i}", [128, 8], F32) for i in range(2)]
                wabs = SB(ph, "wabs", [128, 8], F32)
                wsgn = SB(ph, "wsgn", [128, 8], F32)
                Rb = [SB(ph, f"Rb{i}", [128, 512], F32) for i in range(2)]
                mid = SB(ph, "mid", [128, 1], F32)
                cntb = SB(ph, "cntb", [128, 1], F32)
                cnt2 = SB(ph, "cnt2", [128, 1], F32)
                ajunk = SB(ph, "ajunk", [128, S - max(128, (int(S * 0.45) // 128) * 128)], BF16)
                tmpb = SB(ph, "tmpb", [128, 1], F32)
                mk = [SB(ph, f"mk{i}", [128, 512], BF16) for i in range(2)]
                mkT = [SB(ph, f"mkT{i}", [128, 512], BF16) for i in range(2)]
                Kc = [SB(ph, f"Kc{i}", [128, 8, 512], BF16) for i in range(3)]
                Vc = [SB(ph, f"Vc{i}", [128, 4, 8, 129], BF16) for i in range(3)]
                Eb = [SB(ph, f"Eb{i}", [128, 512], BF16) for i in range(4)]
                Pb = [SB(ph, f"Pb{i}", [128, 512], BF16) for i in range(4)]
                ysb = SB(ph, "ysb", [128, 1024], BF16)
                rz = SB(ph, "rz", [128, 8], F32)
                yTb = [SB(ph, f"yTb{i}", [128, 8, 128], BF16) for i in range(2)]
                ps_idx = [pb[0], pb[1]]
                ps_s = [pb[0], pb[1], pb[2], pb[3]]
                ps_o = [pb[4], pb[5], pb[6]]
                WSC = (8.0 ** -0.5) * (64.0 ** -0.5)
                ASC = 128.0 ** -0.5
                n_i = 0
                n_c = 0
                for i in range(NO):
                    L = (i + 1) * 512
                    nch = L // 512
                    q_, qi_, w_ = qTb[i % 2], qiTb[i % 2], wib[i % 2]
                    k.dma("sp", q_[:], qT[i], writes=[q_])
                    k.dma("sp", qi_[:], qiT[i], writes=[qi_])
                    k.dma("sp", w_[:], wi_s[i], writes=[w_])
                    k.op("act", lambda: nc.scalar.activation(out=wabs[:], in_=w_[:], func=AF.Abs, scale=WSC),
                         [w_], [wabs])
                    k.op("act", lambda: nc.scalar.activation(out=wsgn[:], in_=w_[:], func=AF.Sign), [w_], [wsgn])
                    for c in range(nch):
                        sl = slice(c * 512, (c + 1) * 512)
                        for h in range(8):
                            ps = ps_idx[n_i % 2]
                            R = Rb[n_i % 2]
                            n_i += 1
                            mm(ps, ps[:, :], qi_[:, h, :], kiT[0:64, sl], True, True, [qi_, kiT])
                            k.op("act", lambda ps=ps, R=R, h=h: nc.scalar.activation(
                                out=R[:], in_=ps[:, :], func=AF.Relu, scale=wabs[:, h:h + 1]), [ps, wabs], [R])
                            if h == 0:
                                k.op("dve", lambda R=R, sl=sl: nc.vector.tensor_scalar(
                                    out=score[:, sl], in0=R[:], scalar1=wsgn[:, 0:1], scalar2=None, op0=OP.mult),
                                    [R, wsgn], [score])
                            else:
                                k.op("dve", lambda R=R, sl=sl, h=h: nc.vector.scalar_tensor_tensor(
                                    out=score[:, sl], in0=R[:], scalar=wsgn[:, h:h + 1], in1=score[:, sl],
                                    op0=OP.mult, op1=OP.add), [R, wsgn, score], [score])
                    k.op("dve", lambda: nc.vector.tensor_tensor(out=score[:, L - 512:L], in0=score[:, L - 512:L],
                                                                in1=cmask[:], op=OP.add), [score, cmask], [score])
                    H1 = max(128, (int(L * 0.45) // 128) * 128)
                    n2 = float(L - H1)
                    k.op("dve", lambda: nc.vector.memset(mid[:], 3.14159e-7), [], [mid])
                    for it in range(NBIS):
                        hk = 16.0 * (2.0 ** -it)
                        last = it == NBIS - 1
                        k.op("dve", lambda: nc.vector.tensor_scalar(
                            out=junk[:, 0:1].broadcast_to([128, H1]), in0=score[:, 0:H1], scalar1=mid[:, 0:1],
                            scalar2=0.0, op0=OP.is_ge, op1=OP.add, accum_out=cntb[:, 0:1]), [score, mid],
                            [junk, cntb])
                        k.op("act", lambda: nc.scalar.activation(
                            out=ajunk[:, 0:L - H1], in_=score[:, H1:L], func=AF.Sign, bias=mid[:, 0:1], scale=-1.0,
                            accum_out=cnt2[:, 0:1]), [score, mid], [ajunk, cnt2])
                        k.op("dve", lambda: nc.vector.scalar_tensor_tensor(
                            out=tmpb[:], in0=cntb[:], scalar=2.0, in1=cnt2[:], op0=OP.mult, op1=OP.subtract),
                            [cntb, cnt2], [tmpb])
                        step = (hk / 2.0) if not last else hk
                        mul = 2.0 * step if not last else step
                        k.op("dve", lambda mul=mul: nc.vector.tensor_scalar(
                            out=tmpb[:], in0=tmpb[:], scalar1=511.0 - n2, scalar2=mul, op0=OP.is_ge, op1=OP.mult),
                            [tmpb], [tmpb])
                        k.op("dve", lambda step=step: nc.vector.scalar_tensor_tensor(
                            out=mid[:], in0=tmpb[:], scalar=-step, in1=mid[:], op0=OP.add, op1=OP.add),
                            [tmpb, mid], [mid])
                    for b3 in ps_o:
                        k.op("dve", lambda b3=b3: nc.vector.memset(b3[:, :], 0.0), [], [b3])
                    def load(c):
                        sl = slice(c * 512, (c + 1) * 512)
                        K_, V_ = Kc[c % 3], Vc[c % 3]
                        k.dma("sp", K_[:], KT.rearrange("h p s -> p h s")[:, :, sl], writes=[K_])
                        k.dma("sp", V_[:].rearrange("p b h d -> p b (h d)"),
                              Vs[c * 512:(c + 1) * 512, :].rearrange("(b p) e -> p b e", p=128), writes=[V_])

                    def prep(c):
                        sl = slice(c * 512, (c + 1) * 512)
                        m_, mT_ = mk[c % 2], mkT[c % 2]
                        if c + 1 < nch:
                            load(c + 1)
                        k.op("dve", lambda: nc.vector.tensor_scalar(
                            out=m_[:], in0=score[:, sl], scalar1=mid[:, 0:1], scalar2=None, op0=OP.is_ge),
                            [score, mid], [m_])
                        for b in range(4):
                            transp(ptr, ptr[:, b * 128:(b + 1) * 128], m_[:, b * 128:(b + 1) * 128], ident, [m_])
                        evac(mT_[:], ptr[:, 0:512], [ptr], [mT_])

                    def qk(c, h):
                        ps = ps_s[h % 4]
                        K_ = Kc[c % 3]
                        for b in range(4):
                            mm(ps, ps[:, b * 128:(b + 1) * 128], K_[:, h, b * 128:(b + 1) * 128], q_[:, h, :],
                               True, True, [K_, q_])

                    def rest(c, h):
                        ps = ps_s[h % 4]
                        V_, mT_ = Vc[c % 3], mkT[c % 2]
                        E_, P_ = Eb[h % 4], Pb[h % 4]
                        k.op("act", lambda: nc.scalar.activation(
                            out=E_[:], in_=ps[:, :], func=AF.Exp, scale=ASC), [ps], [E_])
                        k.op("dve", lambda: nc.vector.tensor_tensor(
                            out=P_[:], in0=E_[:], in1=mT_[:], op=OP.mult), [E_, mT_], [P_])
                        po = ps_o[h // 3]
                        oc = (h % 3) * 129
                        for b in range(4):
                            mm_acc(po, po[:, oc:oc + 129], P_[:, b * 128:(b + 1) * 128], V_[:, b, h, :],
                                   [P_, V_], inc=(b == 3))

                    items = [(c, h) for c in range(nch) for h in range(8)]
                    DEPTH = 3
                    prepped = set()

                    def issue_qk(j):
                        if j < len(items):
                            c2, h2 = items[j]
                            if c2 not in prepped:
                                prepped.add(c2)
                                prep(c2)
                            qk(c2, h2)

                    load(0)
                    for j in range(DEPTH):
                        issue_qk(j)
                    for idx, (c, h) in enumerate(items):
                        issue_qk(idx + DEPTH)
                        rest(c, h)
                    for h in range(8):
                        po = ps_o[h // 3]
                        oc = (h % 3) * 129
                        k.op("dve", lambda po=po, oc=oc, h=h: nc.vector.reciprocal(
                            out=rz[:, h:h + 1], in_=po[:, oc + 128:oc + 129]), [po], [rz])
                        k.op("dve", lambda po=po, oc=oc, h=h: nc.vector.tensor_scalar(
                            out=ysb[:, h * 128:(h + 1) * 128], in0=po[:, oc:oc + 128], scalar1=rz[:, h:h + 1],
                            scalar2=None, op0=OP.mult), [po, rz], [ysb])
                    yT_ = yTb[i % 2]
                    for h in range(8):
                        transp(ptr, ptr[:, h * 128:(h + 1) * 128], ysb[:, h * 128:(h + 1) * 128], ident, [ysb],
                               inc=(h == 7))
                    evac(yT_[:], ptr[:].rearrange("p (h t) -> p h t", h=8), [ptr], [yT_])
                    k.dma("pool", yaT[i], yT_[:], reads=[yT_])
            k.barrier()

        if "2b" in phases:
            with ExitStack() as ph:
                stg = [SB(ph, f"c_s{i}", [128, 2048], F32) for i in range(2)]
                wa = SB(ph, "wa", [128, 8, D], BF16)
                wb = SB(ph, "wb", [128, 8, D], BF16)
                load_w(stg, wa, 0, w_ba, 2048)
                load_w(stg, wb, 0, w_bg, 2048)
                wsT = SB(ph, "wsT", [128, 8, 128], BF16)
                trilb = SB(ph, "trilb", [128, 128], F32)
                k.dma("sp", trilb[:], tril_d, writes=[trilb])
                for g in range(8):
                    sg = stg[g % 2]
                    k.dma("sp", sg[:, 0:128], wsT_d[g], writes=[sg])
                    k.op("dve", lambda g=g, sg=sg: nc.vector.tensor_tensor(out=wsT[:, g, :], in0=sg[:, 0:128],
                                                                           in1=trilb[:], op=OP.mult),
                         [sg, trilb], [wsT])
                bT = SB(ph, "bT", [128, 8], F32)
                k.dma("sp", bT[:], bT_d, writes=[bT])
                ggm = SB(ph, "ggm", [128, 1024], F32)
                k.dma("sp", ggm[:], g_gm_d, writes=[ggm])
                gub = [SB(ph, f"gub{i}", [128, 1024], BF16) for i in range(2)]
                gvb = [SB(ph, f"gvb{i}", [128, 1024], BF16) for i in range(2)]
                sab = [SB(ph, f"sab{i}", [128, D], BF16) for i in range(2)]
                sbb = [SB(ph, f"sbb{i}", [128, D], BF16) for i in range(2)]
                yab = [SB(ph, f"yab{i}", [128, 8, 128], BF16) for i in range(2)]
                vsq = SB(ph, "vsq", [128, 1024], BF16)
                vss = SB(ph, "vss", [128, 1], F32)
                vrs = SB(ph, "vrs", [128, 1], F32)
                vn = SB(ph, "vn", [128, 1024], BF16)
                yb = SB(ph, "yb", [128, 1024], BF16)
                ybT = SB(ph, "ybT", [128, 8, 128], BF16)
                mixa = SB(ph, "mixa", [128, D], F32)
                mixc = SB(ph, "mixc", [128, D], F32)
                mixb = SB(ph, "mixb", [128, D], BF16)
                mixTb = [SB(ph, f"mixT{i}", [128, KC, 128], BF16) for i in range(2)]
                n_e = 0
                for i in range(NO):
                    gu_, gv_, sa_, sb_, ya_ = gub[i % 2], gvb[i % 2], sab[i % 2], sbb[i % 2], yab[i % 2]
                    mixT = mixTb[i % 2]
                    k.dma("sp", gu_[:], gu_s[i], writes=[gu_])
                    k.dma("sp", gv_[:], gv_s[i], writes=[gv_])
                    k.dma("sp", sa_[:], sga_s[i], writes=[sa_])
                    k.dma("sp", sb_[:], sgb_s[i], writes=[sb_])
                    k.dma("sp", ya_[:], yaT[i], writes=[ya_])
                    k.op("act", lambda gv_=gv_: nc.scalar.activation(out=vsq[:], in_=gv_[:], func=AF.Square,
                                                                    accum_out=vss[:, 0:1]), [gv_], [vsq, vss])
                    k.op("dve", lambda: nc.vector.tensor_scalar(out=vrs[:], in0=vss[:, 0:1], scalar1=1.0 / 1024,
                                                                scalar2=EPS, op0=OP.mult, op1=OP.add), [vss], [vrs])
                    k.op("act", lambda: nc.scalar.activation(out=vrs[:], in_=vrs[:], func=AF.Sqrt), [vrs], [vrs])
                    k.op("dve", lambda: nc.vector.reciprocal(out=vrs[:], in_=vrs[:]), [vrs], [vrs])
                    k.op("dve", lambda gv_=gv_: nc.vector.scalar_tensor_tensor(
                        out=vn[:], in0=gv_[:], scalar=vrs[:, 0:1], in1=ggm[:], op0=OP.mult, op1=OP.mult),
                        [gv_, vrs, ggm], [vn])
                    pz = [pb[0], pb[1]]
                    for g in range(8):
                        ps = pz[g // 4]
                        mm(ps, ps[:, (g % 4) * 128:(g % 4 + 1) * 128], wsT[:, g, :], vn[:, g * 128:(g + 1) * 128],
                           True, True, [wsT, vn], inc=(g % 4 == 3))
                    for g in range(8):
                        ps = pz[g // 4]
                        k.op("dve", lambda ps=ps, g=g, gu_=gu_: nc.vector.scalar_tensor_tensor(
                            out=yb[:, g * 128:(g + 1) * 128], in0=ps[:, (g % 4) * 128:(g % 4 + 1) * 128],
                            scalar=bT[:, g:g + 1], in1=gu_[:, g * 128:(g + 1) * 128], op0=OP.add, op1=OP.mult),
                            [ps, bT, gu_], [yb])
                    for g in range(8):
                        transp(ptr, ptr[:, g * 128:(g + 1) * 128], yb[:, g * 128:(g + 1) * 128], ident, [yb],
                               inc=(g == 7))
                    evac(ybT[:], ptr[:].rearrange("p (g t) -> p g t", g=8), [ptr], [ybT])
                    for cc in range(4):
                        csl = slice(cc * 512, (cc + 1) * 512)
                        psa = pb[2 + (n_e % 2)]
                        psb = pb[4 + (n_e % 2)]
                        n_e += 1
                        for kc in range(8):
                            mm(psa, psa[:, :], ya_[:, kc, :], wa[:, kc, csl], kc == 0, kc == 7, [ya_, wa])
                        for kc in range(8):
                            mm(psb, psb[:, :], ybT[:, kc, :], wb[:, kc, csl], kc == 0, kc == 7, [ybT, wb])
                        k.op("dve", lambda psa=psa, csl=csl, sa_=sa_: nc.vector.tensor_tensor(
                            out=mixa[:, csl], in0=psa[:, :], in1=sa_[:, csl], op=OP.mult), [psa, sa_], [mixa])
                        k.op("dve", lambda psb=psb, csl=csl, sb_=sb_: nc.vector.tensor_tensor(
                            out=mixc[:, csl], in0=psb[:, :], in1=sb_[:, csl], op=OP.mult), [psb, sb_], [mixc])
                        k.op("pool", lambda csl=csl: nc.gpsimd.tensor_tensor(
                            out=mixb[:, csl], in0=mixc[:, csl], in1=mixa[:, csl], op=OP.add), [mixa, mixc], [mixb])
                    for half in range(2):
                        for j in range(8):
                            kc = half * 8 + j
                            transp(ptr, ptr[:, j * 128:(j + 1) * 128], mixb[:, kc * 128:(kc + 1) * 128], ident,
                                   [mixb], inc=(j == 7))
                        evac(mixT[:, half * 8:half * 8 + 8, :], ptr[:].rearrange("p (j t) -> p j t", j=8), [ptr],
                             [mixT])
                    k.dma("pool", mixT_s[i], mixT[:], reads=[mixT])
            k.barrier()

        if "2c" in phases:
            with ExitStack() as ph:
                stg = [SB(ph, f"c2_s{i}", [128, 2048], F32) for i in range(2)]
                wo = SB(ph, "wo", [128, KC, D], BF16)
                load_w(stg, wo, 0, w_o, 2048)
                xob = [SB(ph, f"xob{i}", [128, D], F32) for i in range(2)]
                mixTb = [SB(ph, f"c2_mT{i}", [128, KC, 128], BF16) for i in range(2)]
                n_e = 0
                for i in range(NO):
                    xo_, mixT = xob[i % 2], mixTb[i % 2]
                    k.dma("sp", xo_[:], x_own[i * 128:(i + 1) * 128, :], writes=[xo_])
                    k.dma("sp", mixT[:], mixT_s[i], writes=[mixT])
                    for cc in range(4):
                        csl = slice(cc * 512, (cc + 1) * 512)
                        ps = pb[n_e % 4]
                        n_e += 1
                        for kc in range(KC):
                            mm(ps, ps[:, :], mixT[:, kc, :], wo[:, kc, csl], kc == 0, kc == KC - 1, [mixT, wo])
                        k.op("dve", lambda ps=ps, csl=csl, xo_=xo_: nc.vector.tensor_tensor(
                            out=xo_[:, csl], in0=ps[:, :], in1=xo_[:, csl], op=OP.add), [ps, xo_], [xo_])
                    k.dma("pool", x1_s[i], xo_[:], reads=[xo_])
            k.barrier()

        if "3a" in phases:
            with ExitStack() as ph:
                stg = [SB(ph, f"d_s{i}", [128, 2048], F32) for i in range(2)]
                wq = SB(ph, "wq", [128, KC, D], BF16)
                load_w(stg, wq, 0, w_pq, 2048)
                skT = SB(ph, "skT", [128, 16, 128], BF16)
                for b in range(16):
                    sg = stg[b % 2]
                    k.dma("sp", sg[:, 0:128], skT_d[b], writes=[sg])
                    cast("dve", skT[:, b, :], sg[:, 0:128], [sg], [skT])
                gbc = SB(ph, "gbc3", [128, D], F32)
                k.dma("sp", gbc[:], g_ffn_d, writes=[gbc])
                xin = [SB(ph, f"d_x{i}", [128, D], F32) for i in range(2)]
                sqj = SB(ph, "d_sqj", [128, D], BF16)
                hn = SB(ph, "d_hn", [128, D], BF16)
                ssq = SB(ph, "d_ssq", [128, 1], F32)
                rstd = SB(ph, "d_rstd", [128, 1], F32)
                hTbs = [SB(ph, f"d_hT{i}", [128, KC, 128], BF16) for i in range(2)]
                qpT = SB(ph, "qpT", [128, 16, 128], BF16)
                ssbs = [SB(ph, f"ssb{i}", [128, 16, 128], F32) for i in range(2)]
                n_e = 0
                for i in range(NO):
                    xt = xin[i % 2]
                    hTb = hTbs[i % 2]
                    ssb = ssbs[i % 2]
                    k.dma("sp", xt[:], x1_s[i], writes=[xt])
                    rms_rows(ph, xt, ssq, rstd, sqj)
                    norm_transpose(xt, gbc, rstd, hn, hTb, lambda k0, n, hTb=hTb: hTb[:, k0:k0 + n, :])
                    k.dma("pool", hn2T[i], hTb[:], reads=[hTb])
                    for bq in range(4):
                        ps = pb[n_e % 2]
                        n_e += 1
                        for bb in range(4):
                            blk = bq * 4 + bb
                            for kc in range(KC):
                                mm(ps, ps[:, bb * 128:(bb + 1) * 128], wq[:, kc, blk * 128:(blk + 1) * 128],
                                   hTb[:, kc, :], kc == 0, kc == KC - 1, [wq, hTb], inc=(kc == KC - 1 and bb == 3))
                        evac(qpT[:, bq * 4:bq * 4 + 4, :], ps[:, :].rearrange("p (b t) -> p b t", b=4), [ps], [qpT])
                    for bq in range(4):
                        ps = pb[2 + (n_e % 2)]
                        n_e += 1
                        for bb in range(4):
                            blk = bq * 4 + bb
                            mm(ps, ps[:, bb * 128:(bb + 1) * 128], qpT[:, blk, :], skT[:, blk, :], True, True,
                               [qpT, skT], inc=(bb == 3))
                        k.op("act", lambda ps=ps, bq=bq, ssb=ssb: nc.scalar.copy(
                            out=ssb[:, bq * 4:bq * 4 + 4, :], in_=ps[:, :].rearrange("p (b n) -> p b n", b=4)),
                            [ps], [ssb])
                    k.dma("pool", s_s[i], ssb[:], reads=[ssb])
            k.barrier()

        if "3b" in phases:
            with ExitStack() as ph:
                iota_i = SB(ph, "iota_i", [128, 128], F32)
                k.dma("sp", iota_i[:], iota_d, writes=[iota_i])
                ssb = SB(ph, "ssb", [128, 16, 128], F32)
                top = SB(ph, "top", [128, 16, 16], F32)
                tix = SB(ph, "tix", [128, 16, 16], U32)
                tixf = SB(ph, "tixf", [128, 16, 16], F32)
                cand = SB(ph, "cand", [128, 8, 256], F32)
                best = SB(ph, "best", [128, 8, 16], F32)
                bix = SB(ph, "bix", [128, 8, 16], U32)
                ba_u = SB(ph, "ba_u", [128, 8, 16], U32)
                bb_u = SB(ph, "bb_u", [128, 8, 16], U32)
                ba_f = SB(ph, "ba_f", [128, 8, 16], F32)
                bb_f = SB(ph, "bb_f", [128, 8, 16], F32)
                eq = SB(ph, "eq", [128, 8, 16, 16], F32)
                isel = SB(ph, "isel", [128, 8, 16], F32)
                jsel = SB(ph, "jsel", [128, 8, 16], F32)
                gate = SB(ph, "gate", [128, 8, 16], F32)
                gsum = SB(ph, "gsum", [128, 8], F32)
                iT = SB(ph, "iT", [128, 128], F32)
                jT = SB(ph, "jT", [128, 128], F32)
                gT = SB(ph, "gT", [128, 128], F32)
                A_all = SB(ph, "A_all", [128, 128, 128], BF16)
                B_all = SB(ph, "B_all", [128, 128, 128], BF16)
                Gt = [SB(ph, f"Gt{i}", [128, 128, 128], BF16) for i in range(1)]
                for i in range(NO):
                    k.dma("sp", ssb[:], s_s[i], writes=[ssb])
                    for blk in range(16):
                        k.op("dve", lambda blk=blk: nc.vector.max(out=top[:, blk, 0:8], in_=ssb[:, blk, :]),
                             [ssb], [top])
                        k.op("dve", lambda blk=blk: nc.vector.max_index(out=tix[:, blk, 0:8], in_max=top[:, blk, 0:8],
                                                                        in_values=ssb[:, blk, :]), [ssb, top], [tix])
                        k.op("dve", lambda blk=blk: nc.vector.match_replace(
                            out=ssb[:, blk, :], in_to_replace=top[:, blk, 0:8], in_values=ssb[:, blk, :],
                            imm_value=NEG), [ssb, top], [ssb])
                        k.op("dve", lambda blk=blk: nc.vector.max(out=top[:, blk, 8:16], in_=ssb[:, blk, :]),
                             [ssb], [top])
                        k.op("dve", lambda blk=blk: nc.vector.max_index(out=tix[:, blk, 8:16],
                                                                        in_max=top[:, blk, 8:16],
                                                                        in_values=ssb[:, blk, :]),
                             [ssb, top], [tix])
                    k.op("dve", lambda: nc.vector.tensor_copy(out=tixf[:], in_=tix[:]), [tix], [tixf])
                    topv = top[:].rearrange("p (h two) k -> p h two k", two=2)
                    k.op("dve", lambda: nc.vector.tensor_tensor(
                        out=cand[:].rearrange("p h (a b) -> p h a b", a=16),
                        in0=topv[:, :, 0, :].unsqueeze(3).broadcast_to([128, 8, 16, 16]),
                        in1=topv[:, :, 1, :].unsqueeze(2).broadcast_to([128, 8, 16, 16]), op=OP.add), [top], [cand])
                    for h in range(8):
                        k.op("dve", lambda h=h: nc.vector.max(out=best[:, h, 0:8], in_=cand[:, h, :]), [cand], [best])
                        k.op("dve", lambda h=h: nc.vector.max_index(out=bix[:, h, 0:8], in_max=best[:, h, 0:8],
                                                                    in_values=cand[:, h, :]), [cand, best], [bix])
                        k.op("dve", lambda h=h: nc.vector.match_replace(
                            out=cand[:, h, :], in_to_replace=best[:, h, 0:8], in_values=cand[:, h, :],
                            imm_value=NEG), [cand, best], [cand])
                        k.op("dve", lambda h=h: nc.vector.max(out=best[:, h, 8:16], in_=cand[:, h, :]),
                             [cand], [best])
                        k.op("dve", lambda h=h: nc.vector.max_index(out=bix[:, h, 8:16], in_max=best[:, h, 8:16],
                                                                    in_values=cand[:, h, :]), [cand, best], [bix])
                    k.op("dve", lambda: nc.vector.tensor_single_scalar(out=ba_u[:], in_=bix[:], scalar=4,
                                                                       op=OP.logical_shift_right), [bix], [ba_u])
                    k.op("dve", lambda: nc.vector.tensor_single_scalar(out=bb_u[:], in_=bix[:], scalar=15,
                                                                       op=OP.bitwise_and), [bix], [bb_u])
                    k.op("dve", lambda: nc.vector.tensor_copy(out=ba_f[:], in_=ba_u[:]), [ba_u], [ba_f])
                    k.op("dve", lambda: nc.vector.tensor_copy(out=bb_f[:], in_=bb_u[:]), [bb_u], [bb_f])
                    tixv = tixf[:].rearrange("p (h two) k -> p h two k", two=2)
                    iota16 = iota_i[:, 0:16].unsqueeze(1).unsqueeze(1).broadcast_to([128, 8, 16, 16])
                    for (sel_f, which, dst) in ((ba_f, 0, isel), (bb_f, 1, jsel)):
                        k.op("dve", lambda sel_f=sel_f: nc.vector.tensor_tensor(
                            out=eq[:], in0=sel_f[:].unsqueeze(3).broadcast_to([128, 8, 16, 16]), in1=iota16,
                            op=OP.is_equal), [sel_f, iota_i], [eq])
                        k.op("dve", lambda which=which: nc.vector.tensor_tensor(
                            out=eq[:], in0=eq[:], in1=tixv[:, :, which, :].unsqueeze(2).broadcast_to([128, 8, 16, 16]),
                            op=OP.mult), [eq, tixf], [eq])
                        k.op("dve", lambda dst=dst: nc.vector.tensor_reduce(out=dst[:], in_=eq[:], axis=AX.X,
                                                                            op=OP.add), [eq], [dst])
                    k.op("dve", lambda: nc.vector.tensor_tensor(
                        out=gate[:], in0=best[:], in1=best[:, :, 0:1].broadcast_to([128, 8, 16]), op=OP.subtract),
                        [best], [gate])
                    k.op("act", lambda: nc.scalar.activation(out=gate[:], in_=gate[:], func=AF.Exp), [gate], [gate])
                    k.op("dve", lambda: nc.vector.tensor_reduce(out=gsum[:], in_=gate[:], axis=AX.X, op=OP.add),
                         [gate], [gsum])
                    k.op("dve", lambda: nc.vector.reciprocal(out=gsum[:], in_=gsum[:]), [gsum], [gsum])
                    k.op("dve", lambda: nc.vector.tensor_tensor(
                        out=gate[:], in0=gate[:], in1=gsum[:].unsqueeze(2).broadcast_to([128, 8, 16]), op=OP.mult),
                        [gate, gsum], [gate])
                    pt_f = pb[4]
                    for (src, dst, col) in ((isel, iT, 0), (jsel, jT, 1), (gate, gT, 2)):
                        k.op("pe", lambda src=src, col=col: nc.tensor.transpose(
                            out=pt_f[:, col * 128:(col + 1) * 128], in_=src[:].rearrange("p h k -> p (h k)"),
                            identity=identf[:]), [src, identf], [pt_f], skip_self=True)
                        evac(dst[:], pt_f[:, col * 128:(col + 1) * 128], [pt_f], [dst])
                    iota_b = iota_i[:].unsqueeze(1).broadcast_to([128, 128, 128])
                    k.op("dve", lambda: nc.vector.tensor_tensor(
                        out=A_all[:], in0=iota_b, in1=iT[:].unsqueeze(2).broadcast_to([128, 128, 128]),
                        op=OP.is_equal), [iota_i, iT], [A_all])
                    k.op("pool", lambda: nc.gpsimd.tensor_tensor(
                        out=A_all[:], in0=A_all[:], in1=gT[:].unsqueeze(2).broadcast_to([128, 128, 128]),
                        op=OP.mult), [A_all, gT], [A_all])
                    k.op("dve", lambda: nc.vector.tensor_tensor(
                        out=B_all[:], in0=iota_b, in1=jT[:].unsqueeze(2).broadcast_to([128, 128, 128]),
                        op=OP.is_equal), [iota_i, jT], [B_all])
                    G_ = Gt[0]
                    for t4 in range(32):
                        ps = pb[5 + (t4 % 2)]
                        for tt in range(4):
                            t = t4 * 4 + tt
                            mm(ps, ps[:, tt * 128:(tt + 1) * 128], B_all[:, t, :], A_all[:, t, :], True, True,
                               [A_all, B_all], inc=(tt == 3))
                        k.op("act", lambda ps=ps, t4=t4: nc.scalar.copy(
                            out=G_[:, :, t4 * 4:t4 * 4 + 4].rearrange("p i t -> p t i"),
                            in_=ps[:, :].rearrange("p (t i) -> p t i", t=4)), [ps], [G_])
                    for q4 in range(4):
                        k.dma("pool", G_s[i][:, q4 * 32:(q4 + 1) * 32, :], G_[:, q4 * 32:(q4 + 1) * 32, :], reads=[G_])
            k.barrier()

        if "4" in phases:
            TT = min(4, NO)
            NTT = NO // TT
            TW = TT * 128
            GB = 4
            with ExitStack() as ph:
                gfin = SB(ph, "gfin", [128, D], F32)
                k.dma("sp", gfin[:], g_fin_d, writes=[gfin])
                hT4 = SB(ph, "hT4", [128, KC, TW], BF16)
                acc = SB(ph, "acc", [128, TT, D], F32)
                Ug = [SB(ph, f"Ug{i}", [128, KC, GB * 128], BF16) for i in range(2)]
                Vg = [SB(ph, f"Vg{i}", [128, GB, D], BF16) for i in range(2)]
                Gg = [SB(ph, f"Gg{i}", [128, GB, TW], BF16) for i in range(2)]
                GA = [SB(ph, f"GA{i}", [128, GB, TW], BF16) for i in range(2)]
                gl = [SB(ph, f"gl{i}", [128, TW], BF16) for i in range(2)]
                sqj = SB(ph, "e_sqj", [128, D], BF16)
                ssq = SB(ph, "e_ssq", [128, 1], F32)
                rstd = SB(ph, "e_rstd", [128, 1], F32)
                ob = [SB(ph, f"e_ob{i}", [128, D], F32) for i in range(2)]
                n_g = 0
                n_e = 0
                for tt_i in range(NTT):
                    for s_ in range(TT):
                        i = tt_i * TT + s_
                        k.dma("sp", hT4[:, :, s_ * 128:(s_ + 1) * 128], hn2T[i], writes=[hT4])
                        k.dma("sp", acc[:, s_, :], x1_s[i], writes=[acc])
                    for g in range(128 // GB):
                        U_, V_, G_, A_ = Ug[n_g % 2], Vg[n_g % 2], Gg[n_g % 2], GA[n_g % 2]
                        n_g += 1
                        e0 = g * GB * 128
                        k.dma("sp", U_[:], Ubf[:, e0:e0 + GB * 128].rearrange("(kc p) e -> p kc e", p=128),
                              writes=[U_])
                        k.dma("sp", V_[:], Vbf[e0:e0 + GB * 128, :].rearrange("(b p) d -> p b d", p=128),
                              writes=[V_])
                        for s_ in range(TT):
                            i = tt_i * TT + s_
                            k.dma("sp", G_[:, :, s_ * 128:(s_ + 1) * 128], G_s[i][:, g * GB:(g + 1) * GB, :],
                                  writes=[G_])
                        for b in range(GB):
                            ps = pb[n_e % 2]
                            n_e += 1
                            for kc in range(KC):
                                mm(ps, ps[:, 0:TW], U_[:, kc, b * 128:(b + 1) * 128], hT4[:, kc, :], kc == 0,
                                   kc == KC - 1, [U_, hT4])
                            g_ = gl[b % 2]
                            k.op("act", lambda ps=ps, g_=g_: nc.scalar.activation(
                                out=g_[:], in_=ps[:, 0:TW], func=AF.Gelu_apprx_tanh), [ps], [g_])
                            k.op("dve", lambda g_=g_, b=b, A_=A_, G_=G_: nc.vector.tensor_tensor(
                                out=A_[:, b, :], in0=g_[:], in1=G_[:, b, :], op=OP.mult), [g_, G_], [A_])
                        for s_ in range(TT):
                            for cc in range(4):
                                csl = slice(cc * 512, (cc + 1) * 512)
                                ps = pb[2 + (n_e % 4)]
                                n_e += 1
                                for b in range(GB):
                                    mm(ps, ps[:, :], A_[:, b, s_ * 128:(s_ + 1) * 128], V_[:, b, csl], b == 0,
                                       b == GB - 1, [A_, V_])
                                k.op("dve", lambda ps=ps, s_=s_, csl=csl: nc.vector.tensor_tensor(
                                    out=acc[:, s_, csl], in0=ps[:, :], in1=acc[:, s_, csl], op=OP.add),
                                    [ps, acc], [acc])
                    for s_ in range(TT):
                        i = tt_i * TT + s_
                        o_ = ob[s_ % 2]
                        k.op("act", lambda s_=s_: nc.scalar.activation(out=sqj[:], in_=acc[:, s_, :], func=AF.Square,
                                                                       accum_out=ssq[:, 0:1]), [acc], [sqj, ssq])
                        k.op("dve", lambda: nc.vector.tensor_scalar(out=rstd[:], in0=ssq[:, 0:1], scalar1=1.0 / D,
                                                                    scalar2=EPS, op0=OP.mult, op1=OP.add),
                             [ssq], [rstd])
                        k.op("act", lambda: nc.scalar.activation(out=rstd[:], in_=rstd[:], func=AF.Sqrt),
                             [rstd], [rstd])
                        k.op("dve", lambda: nc.vector.reciprocal(out=rstd[:], in_=rstd[:]), [rstd], [rstd])
                        k.op("dve", lambda s_=s_, o_=o_: nc.vector.scalar_tensor_tensor(
                            out=o_[:], in0=acc[:, s_, :], scalar=rstd[:, 0:1], in1=gfin[:], op0=OP.mult, op1=OP.mult),
                            [acc, rstd, gfin], [o_])
                        k.dma("pool", out_d[i * 128:(i + 1) * 128, :], o_[:], reads=[o_])
            k.barrier()
        else:
            pass
        k.barrier()
        nc._kb_inst = k.n_inst
        nc._kb_want = k.want
    return nc


def build2(S, phases=None, expose=()):
    dry = build(S, phases, expose, None)
    return build(S, phases, expose, dry._kb_want)


def _host_inputs(inp, S, n_cores=8, phases=None):
    x = np.asarray(inp["x"], np.float32)
    B = x.shape[0]
    NT = S // 128
    NO = NT // 4
    f = lambda a: np.ascontiguousarray(np.asarray(a, np.float32))
    bc = lambda v, n: f(np.broadcast_to(np.asarray(v, np.float32).reshape(1, n), (128, n)))
    common = {
        "tril": f(np.triu(np.ones((128, 128), np.float32))),
        "ident": f(np.eye(128, dtype=np.float32)),
        "iota": f(np.broadcast_to(np.arange(128, dtype=np.float32)[None, :], (128, 128))),
        "g_mix": bc(inp["ln_mix_g"][0], D),
        "g_ffn": bc(inp["ln_ffn_g"][0], D),
        "g_fin": bc(inp["ln_final_g"], D),
        "g_gm": bc(inp["gmlp_norm_g"][0], 1024),
        "w_in": f(inp["w_in"][0]),
        "wsT": f(np.transpose(inp["w_spatial"][0], (0, 2, 1))),
        "bT": f(np.transpose(inp["b_spatial"][0], (1, 0))),
        "w_ba": f(inp["w_branch_attn"][0]),
        "w_bg": f(inp["w_branch_gmlp"][0]),
        "w_o": f(inp["w_out"][0]),
        "w_pq": f(inp["peer_w_q"][0]),
        "skT": f(np.transpose(np.asarray(inp["peer_sub_keys"][0]).reshape(16, 128, 128), (0, 2, 1))),
    }
    if phases is None or "0" in phases:
        common["uT"] = f(np.asarray(inp["peer_u"][0]).T)
        common["pv"] = f(inp["peer_v"][0])
    maps = []
    for c in range(n_cores):
        b, r = c // 4, c % 4
        xb = x[b % B, :S]
        own = np.concatenate([xb[(4 * i + r) * 128:(4 * i + r + 1) * 128] for i in range(NO)], axis=0)
        sp = np.arange(512)[None, :]
        tq = np.arange(128)[:, None]
        cm = np.where(sp <= r * 128 + tq, 0.0, NEG).astype(np.float32)
        m = dict(common)
        m["x_all"] = f(xb)
        m["x_own"] = f(own)
        m["cmask"] = f(cm)
        maps.append(m)
    return maps


def _gather(results, S, B=2):
    NT = S // 128
    NO = NT // 4
    out = np.zeros((B, S, D), np.float32)
    for c, rmap in enumerate(results):
        b, r = c // 4, c % 4
        o = np.asarray(rmap["out"])
        for i in range(NO):
            out[b, (4 * i + r) * 128:(4 * i + r + 1) * 128] = o[i * 128:(i + 1) * 128]
    return out


def kernel(**inputs):
    S = inputs["x"].shape[1]
    nc = build2(S)
    maps = _host_inputs(inputs, S)
    res = run_bass_kernel_spmd(nc, maps, core_ids=list(range(8)))
    return _gather(res.results, S)
```
